# Optimizing a Trainium2 kernel written in Bass

```python
import math
import jax, jax.numpy as jnp
from jax import lax
import numpy as np

D_MODEL = 1024
BATCH = 16
SEQ = 2048
DEPTH = 2

HEAD_DIM = 64
ATTN_WIDTH = D_MODEL // 2
ATTN_HEADS = ATTN_WIDTH // HEAD_DIM
DILATED_CONFIGS = ((128, 1), (512, 4), (2048, 16))
ROPE_THETA = 500000.0
ROPE_DIM = HEAD_DIM // 4
GMLP_WIDTH = D_MODEL // 4
GMLP_GROUPS = 4
GMLP_GROUP_DIM = GMLP_WIDTH // GMLP_GROUPS
GMLP_CHUNK = 128
POOL_WIDTH = D_MODEL - ATTN_WIDTH - GMLP_WIDTH
POOL_WINDOWS = (2, 4, 8, 16)
POOL_GROUPS = len(POOL_WINDOWS)
POOL_GROUP_DIM = POOL_WIDTH // POOL_GROUPS
IN_COLS = 3 * ATTN_WIDTH + 2 * GMLP_WIDTH + POOL_WIDTH

N_EXPERTS = 16
N_EXPERT_GROUPS = 4
EXPERTS_PER_GROUP = N_EXPERTS // N_EXPERT_GROUPS
TOP_K = 2
EXPERT_FF = D_MODEL // 2
MOE_BLOCK = 128

DEEPNORM_ALPHA = float((2 * DEPTH) ** 0.25)
DEEPNORM_BETA = float((8 * DEPTH) ** -0.25)
LN_EPS = 1e-5
NEG_INF = -1e30

kernel_name = "hybrid_dilated_gmlp_pool_groupmoe_deepnorm"


def _layernorm(x, g, b):
    xf = x.astype(jnp.float32)
    mu = xf.mean(-1, keepdims=True)
    var = jnp.square(xf - mu).mean(-1, keepdims=True)
    y = (xf - mu) * lax.rsqrt(var + LN_EPS) * g.astype(jnp.float32) + b.astype(jnp.float32)
    return y.astype(x.dtype)


def _partial_rope(x, positions):
    half = ROPE_DIM // 2
    inv_freq = jnp.power(jnp.float32(ROPE_THETA), -jnp.arange(half, dtype=jnp.float32) / half)
    ang = positions.astype(jnp.float32)[:, None] * inv_freq[None, :]
    cos = jnp.cos(ang)[None, :, None, :]
    sin = jnp.sin(ang)[None, :, None, :]
    xr = x[..., :ROPE_DIM].astype(jnp.float32)
    x1, x2 = xr[..., :half], xr[..., half:]
    rot = jnp.concatenate([x1 * cos - x2 * sin, x2 * cos + x1 * sin], axis=-1).astype(x.dtype)
    return jnp.concatenate([rot, x[..., ROPE_DIM:]], axis=-1)


def _banded_window_attention(q, k, v, half):
    lead = q.shape[:-2]
    L, dh = q.shape[-2], q.shape[-1]
    nlead = len(lead)
    blk = half
    nb = -(-L // blk)
    lp = nb * blk
    qb = jnp.pad(q, [(0, 0)] * nlead + [(0, lp - L), (0, 0)]).reshape(*lead, nb, blk, dh)
    kv_pad = [(0, 0)] * nlead + [(blk, lp - L + blk), (0, 0)]

    def band(t):
        tb = jnp.pad(t, kv_pad).reshape(*lead, nb + 2, blk, dh)
        return jnp.concatenate([tb[..., :-2, :, :], tb[..., 1:-1, :, :], tb[..., 2:, :, :]], axis=-2)

    kw = band(k)
    vw = band(v)
    s = jnp.einsum('...nqd,...nkd->...nqk', qb, kw,
                   preferred_element_type=jnp.float32) * (HEAD_DIM ** -0.5)
    a = jnp.arange(blk)[:, None]
    c = jnp.arange(3 * blk)[None, :]
    n = jnp.arange(nb)[:, None, None]
    offset = c - blk - a
    key_idx = (n - 1) * blk + c
    mask = (jnp.abs(offset) <= half) & (key_idx >= 0) & (key_idx < L)
    s = jnp.where(mask, s, NEG_INF)
    m = s.max(-1, keepdims=True)
    p = jnp.exp(s - m)
    l = p.sum(-1, keepdims=True)
    o = jnp.einsum('...nqk,...nkd->...nqd', p.astype(v.dtype), vw,
                   preferred_element_type=jnp.float32) / l
    lse = (m + jnp.log(l))[..., 0]
    o = o.reshape(*lead, lp, dh)[..., :L, :]
    lse = lse.reshape(*lead, lp)[..., :L]
    return o, lse


def _dilated_attention(q, k, v):
    B, H, S, dh = q.shape
    outs, lses = [], []
    for window, dil in DILATED_CONFIGS:
        half = window // (2 * dil)

        def to_residue(t):
            return t.reshape(B, H, S // dil, dil, dh).swapaxes(2, 3)

        o, lse = _banded_window_attention(to_residue(q), to_residue(k), to_residue(v), half)
        outs.append(o.swapaxes(2, 3).reshape(B, H, S, dh))
        lses.append(lse.swapaxes(2, 3).reshape(B, H, S))
    wts = jax.nn.softmax(jnp.stack(lses, axis=0), axis=0)
    out = jnp.einsum('cbhs,cbhsd->bhsd', wts, jnp.stack(outs, axis=0))
    return out.astype(q.dtype)


def _gmlp_mixer(u, v, ln_g, ln_b, w_s, b_s):
    B, S, G, dg = u.shape
    u = jax.nn.gelu(u)
    v = _layernorm(jax.nn.gelu(v), ln_g, ln_b)
    vc = v.reshape(B, S // GMLP_CHUNK, GMLP_CHUNK, G, dg)
    sv = jnp.einsum('gpq,bcqgd->bcpgd', w_s, vc) + b_s.T[None, None, :, :, None]
    return u * sv.reshape(B, S, G, dg)


def _pool_mixer(z, w_pool, scale):
    B, S, G, dg = z.shape
    zf = z.astype(jnp.float32)
    cs = jnp.concatenate([jnp.zeros((B, 1, G, dg), jnp.float32), jnp.cumsum(zf, axis=1)], axis=1)
    pos = jnp.arange(S)
    pooled = []
    for g, w in enumerate(POOL_WINDOWS):
        left = w // 2
        right = w - 1 - left
        lo = jnp.clip(pos - left, 0, S)
        hi = jnp.clip(pos + right + 1, 0, S)
        cnt = (hi - lo).astype(jnp.float32)[None, :, None]
        csg = cs[:, :, g]
        mean = (jnp.take(csg, hi, axis=1) - jnp.take(csg, lo, axis=1)) / cnt
        pooled.append(mean - zf[:, :, g])
    pooled = jnp.stack(pooled, axis=2).astype(z.dtype)
    y = jnp.einsum('bsgi,gio->bsgo', pooled, w_pool)
    return y * scale.reshape(G, dg)


def _mixing_sublayer(x, w_in, w_out, gmlp_ln_g, gmlp_ln_b, gmlp_w_s, gmlp_b_s, pool_w, pool_scale):
    B, S, _ = x.shape
    proj = x @ w_in
    o1 = ATTN_WIDTH
    o2 = 2 * ATTN_WIDTH
    o3 = 3 * ATTN_WIDTH
    o4 = o3 + GMLP_WIDTH
    o5 = o4 + GMLP_WIDTH
    positions = jnp.arange(S)
    q = _partial_rope(proj[..., :o1].reshape(B, S, ATTN_HEADS, HEAD_DIM), positions)
    k = _partial_rope(proj[..., o1:o2].reshape(B, S, ATTN_HEADS, HEAD_DIM), positions)
    v = proj[..., o2:o3].reshape(B, S, ATTN_HEADS, HEAD_DIM)
    attn = _dilated_attention(q.transpose(0, 2, 1, 3), k.transpose(0, 2, 1, 3), v.transpose(0, 2, 1, 3))
    attn = attn.transpose(0, 2, 1, 3).reshape(B, S, ATTN_WIDTH)
    u = proj[..., o3:o4].reshape(B, S, GMLP_GROUPS, GMLP_GROUP_DIM)
    vg = proj[..., o4:o5].reshape(B, S, GMLP_GROUPS, GMLP_GROUP_DIM)
    gm = _gmlp_mixer(u, vg, gmlp_ln_g, gmlp_ln_b, gmlp_w_s, gmlp_b_s).reshape(B, S, GMLP_WIDTH)
    pz = proj[..., o5:].reshape(B, S, POOL_GROUPS, POOL_GROUP_DIM)
    pl = _pool_mixer(pz, pool_w, pool_scale).reshape(B, S, POOL_WIDTH)
    return jnp.concatenate([attn, gm, pl], axis=-1) @ w_out


def _route(x_flat, router_w, router_bias):
    T = x_flat.shape[0]
    scores = jax.nn.sigmoid((x_flat @ router_w).astype(jnp.float32))
    biased = (scores + router_bias.astype(jnp.float32)).reshape(T, N_EXPERT_GROUPS, EXPERTS_PER_GROUP)
    group_score = lax.top_k(biased, TOP_K)[0].sum(-1)
    gsel = jnp.argmax(group_score, axis=-1)
    in_group = biased[jnp.arange(T), gsel]
    _, local = lax.top_k(in_group, TOP_K)
    experts = gsel[:, None] * EXPERTS_PER_GROUP + local
    sel = jnp.take_along_axis(scores, experts, axis=1)
    gates = sel / sel.sum(-1, keepdims=True)
    return experts, gates


def _moe(x, router_w, router_bias, w_gate, w_up, w_down):
    B, S, D = x.shape
    T = B * S
    A = T * TOP_K
    xf = x.reshape(T, D)
    experts, gates = _route(xf, router_w, router_bias)
    e_flat = experts.reshape(A)
    tok_flat = jnp.broadcast_to(jnp.arange(T)[:, None], (T, TOP_K)).reshape(A)
    g_flat = gates.reshape(A)
    order = jnp.argsort(e_flat)
    e_sorted = e_flat[order]
    tok_sorted = tok_flat[order]
    g_sorted = g_flat[order]
    counts = jnp.zeros((N_EXPERTS,), jnp.int32).at[e_flat].add(1)
    padded = ((counts + MOE_BLOCK - 1) // MOE_BLOCK) * MOE_BLOCK
    start = jnp.cumsum(counts) - counts
    pend = jnp.cumsum(padded)
    pstart = pend - padded
    dest = pstart[e_sorted] + (jnp.arange(A) - start[e_sorted])
    P = A + N_EXPERTS * MOE_BLOCK
    n_blocks = P // MOE_BLOCK
    buf = jnp.zeros((P, D), x.dtype).at[dest].set(xf[tok_sorted])
    blk_expert = jnp.minimum(
        jnp.searchsorted(pend, jnp.arange(n_blocks) * MOE_BLOCK, side='right'), N_EXPERTS - 1)

    def expert_block(args):
        xb, e = args
        h = jax.nn.silu(xb @ w_gate[e]) * (xb @ w_up[e])
        return h @ w_down[e]

    y_buf = lax.map(expert_block, (buf.reshape(n_blocks, MOE_BLOCK, D), blk_expert))
    y_sorted = y_buf.reshape(P, D)[dest] * g_sorted[:, None].astype(x.dtype)
    out = jnp.zeros((T, D), x.dtype).at[tok_sorted].add(y_sorted)
    return out.reshape(B, S, D)


def setup_inputs(seed: int = 0) -> dict:
    key = jax.random.key(seed)
    ks = jax.random.split(key, 20)
    f32 = jnp.float32
    nrm = lambda k, shape, s: jax.random.normal(k, shape, f32) * s
    return {
        "x": nrm(ks[0], (BATCH, SEQ, D_MODEL), 1.0),
        "w_in": nrm(ks[1], (DEPTH, D_MODEL, IN_COLS), D_MODEL ** -0.5),
        "w_out": nrm(ks[2], (DEPTH, D_MODEL, D_MODEL), DEEPNORM_BETA * D_MODEL ** -0.5),
        "gmlp_ln_g": 1.0 + nrm(ks[3], (DEPTH, GMLP_GROUPS, GMLP_GROUP_DIM), 0.02),
        "gmlp_ln_b": nrm(ks[4], (DEPTH, GMLP_GROUPS, GMLP_GROUP_DIM), 0.02),
        "gmlp_w_s": nrm(ks[5], (DEPTH, GMLP_GROUPS, GMLP_CHUNK, GMLP_CHUNK), GMLP_CHUNK ** -0.5),
        "gmlp_b_s": 1.0 + nrm(ks[6], (DEPTH, GMLP_GROUPS, GMLP_CHUNK), 0.02),
        "pool_w": nrm(ks[7], (DEPTH, POOL_GROUPS, POOL_GROUP_DIM, POOL_GROUP_DIM), POOL_GROUP_DIM ** -0.5),
        "pool_scale": 1.0 + nrm(ks[8], (DEPTH, POOL_WIDTH), 0.02),
        "ln1_g": 1.0 + nrm(ks[9], (DEPTH, D_MODEL), 0.02),
        "ln1_b": nrm(ks[10], (DEPTH, D_MODEL), 0.02),
        "router_w": nrm(ks[11], (D_MODEL, N_EXPERTS), D_MODEL ** -0.5),
        "router_bias": nrm(ks[12], (N_EXPERTS,), 0.01),
        "w_gate": nrm(ks[13], (DEPTH, N_EXPERTS, D_MODEL, EXPERT_FF), D_MODEL ** -0.5),
        "w_up": nrm(ks[14], (DEPTH, N_EXPERTS, D_MODEL, EXPERT_FF), D_MODEL ** -0.5),
        "w_down": nrm(ks[15], (DEPTH, N_EXPERTS, EXPERT_FF, D_MODEL), DEEPNORM_BETA * EXPERT_FF ** -0.5),
        "ln2_g": 1.0 + nrm(ks[16], (DEPTH, D_MODEL), 0.02),
        "ln2_b": nrm(ks[17], (DEPTH, D_MODEL), 0.02),
    }


def reference(x, w_in, w_out, gmlp_ln_g, gmlp_ln_b, gmlp_w_s, gmlp_b_s, pool_w, pool_scale,
              ln1_g, ln1_b, router_w, router_bias, w_gate, w_up, w_down, ln2_g, ln2_b):
    for l in range(DEPTH):
        h = _mixing_sublayer(x, w_in[l], w_out[l], gmlp_ln_g[l], gmlp_ln_b[l], gmlp_w_s[l],
                             gmlp_b_s[l], pool_w[l], pool_scale[l])
        x = _layernorm(DEEPNORM_ALPHA * x + h, ln1_g[l], ln1_b[l])
        h = _moe(x, router_w, router_bias, w_gate[l], w_up[l], w_down[l])
        x = _layernorm(DEEPNORM_ALPHA * x + h, ln2_g[l], ln2_b[l])
    return x
```

```python
import os
import numpy as np
import ml_dtypes
from contextlib import ExitStack
import concourse.bass as bass
import concourse.mybir as mybir
from concourse.bass_utils import run_bass_kernel_spmd

F32 = mybir.dt.float32
BF16 = mybir.dt.bfloat16
AF = mybir.ActivationFunctionType
ALU = mybir.AluOpType
AX = mybir.AxisListType

S = 2048
D = 1024
NTB = 16
ALPHA = float(4 ** 0.25)
EPS = 1e-5
ENGS = ['pe', 'act', 'dve', 'pool', 'sp']


class StopBuild(Exception):
    pass


class Res:
    __slots__ = ('name', 'w', 'r')

    def __init__(self, name=''):
        self.name = name
        self.w = None
        self.r = {}


class Plan:
    def __init__(self):
        self.q = {e: [] for e in ENGS}
        self.cnt = {e: 0 for e in ENGS}
        self.known = {e: {} for e in ENGS}
        self.dma_keys = []

    def _collect(self, eng, reads, writes, is_dma):
        evs = {}

        def add(k, v, kind):
            if v is None:
                v = self.cnt[k]
            if k == eng and not is_dma:
                if eng == 'pe':
                    return
            if evs.get(k, 0) < v:
                evs[k] = v
        for r in reads:
            if r.w is not None:
                add(r.w[0], r.w[1], 'raw')
        for w in writes:
            if w.w is not None:
                add(w.w[0], w.w[1], 'waw')
            for k, v in w.r.items():
                add(k, v, 'war')
        waits = []
        kn = self.known[eng]
        for k, v in evs.items():
            if kn.get(k, 0) < v:
                waits.append((k, v))
                kn[k] = v
        return waits

    def op(self, eng, fn, reads=(), writes=()):
        waits = self._collect(eng, reads, writes, False)
        self.cnt[eng] += 1
        v = self.cnt[eng]
        self.q[eng].append((waits, fn, (eng, 1)))
        for r in reads:
            if r.r.get(eng, 0) < v:
                r.r[eng] = v
        for w in writes:
            w.w = (eng, v)
            w.r = {}

    def dma(self, eng, fn, key, reads=(), writes=()):
        if key not in self.cnt:
            self.cnt[key] = 0
            self.dma_keys.append(key)
        waits = self._collect(eng, reads, writes, True)
        self.cnt[key] += 16
        self.q[eng].append((waits, fn, (key, 16)))
        for r in reads:
            r.r[key] = None
        for w in writes:
            w.w = (key, None)
            w.r = {}

    def barrier(self):
        snap = dict(self.cnt)
        for e in ENGS:
            waits = []
            kn = self.known[e]
            for k, v in snap.items():
                if v == 0 or (k == e and e in ('pe', 'sp')):
                    continue
                if kn.get(k, 0) < v:
                    waits.append((k, v))
                    kn[k] = v
            self.q[e].append((waits, None, None))

    def emit(self, nc, stack):
        sems = {}
        for k in ENGS + self.dma_keys:
            sems[k] = stack.enter_context(nc.semaphore("s_" + k.replace(':', '_')))
        block = stack.enter_context(nc.Block())
        q = self.q

        def run(e, lst):
            for waits, fn, inc in lst:
                for k, v in waits:
                    e.wait_ge(sems[k], v)
                if fn is not None:
                    fn(e).then_inc(sems[inc[0]], inc[1])

        @block.tensor
        def _(e):
            run(e, q['pe'])

        @block.scalar
        def _(e):
            run(e, q['act'])

        @block.vector
        def _(e):
            run(e, q['dve'])

        @block.gpsimd
        def _(e):
            run(e, q['pool'])

        @block.sync
        def _(e):
            run(e, q['sp'])


def _consts():
    bf = ml_dtypes.bfloat16
    ki = np.arange(128)[:, None]
    qi = np.arange(128)[None, :]
    mA = (ki >= qi).astype(np.float32)
    mB = (ki <= qi).astype(np.float32)
    m3 = (np.abs(ki - qi) <= 64).astype(np.float32)
    masks = np.concatenate([mA, mB, mA, mB,
                            mB[:, 64:], mB[:, 64:],
                            mA[:, :64], mA[:, :64],
                            m3, m3], axis=1)
    masks = masks.astype(bf)
    identb = np.eye(128, dtype=np.float32).astype(bf)
    ident32 = np.eye(128, dtype=np.float32)
    perm = np.zeros((128, 128), np.float32)
    for base in (0, 64):
        for d in range(8):
            perm[base + d + 8, base + d] = -1.0
            perm[base + d - 0, base + d + 8] = 1.0
    perm = perm.astype(bf)
    pos = np.arange(S, dtype=np.float32)
    inv = np.power(np.float32(500000.0), -np.arange(8, dtype=np.float32) / 8).astype(np.float32)
    ang = pos[None, :] * inv[:, None]
    C = np.ones((128, S), np.float32)
    Sn = np.zeros((128, S), np.float32)
    for base in (0, 64):
        C[base:base + 8] = np.cos(ang)
        C[base + 8:base + 16] = np.cos(ang)
        Sn[base:base + 8] = np.sin(ang)
        Sn[base + 8:base + 16] = np.sin(ang)
    onesh = np.zeros((128, 2, 128), np.float32)
    onesh[:, 0, 0:64] = 1.0
    onesh[:, 1, 64:128] = 1.0
    rce = np.ones((128, 2, 16), np.float32)
    wins = {(0, 0): 2, (1, 0): 4, (0, 1): 8, (1, 1): 16}
    for (e, t), w in wins.items():
        left = w // 2
        right = w - 1 - left
        for i in range(8):
            tt = i
            cnt = min(tt + right + 1, S) - max(tt - left, 0)
            rce[e * 64:(e + 1) * 64, t, i] = 1.0 / cnt
            tt = S - 8 + i
            cnt = min(tt + right + 1, S) - max(tt - left, 0)
            rce[e * 64:(e + 1) * 64, t, 8 + i] = 1.0 / cnt
    return dict(c_masks=masks, c_identb=identb, c_ident32=ident32, c_perm=perm,
                c_ropeC=C.astype(bf), c_ropeS=Sn.astype(bf), c_onesh=onesh.astype(bf),
                c_rce=rce)


def build(NSEQ=2, DEPTH=2, dbg=None):
    nc = bass.Bass("TRN2", target_bir_lowering=False)
    dtn = nc.dram_tensor

    def din(name, shape, dt=F32):
        return dtn(name, list(shape), dt, kind="ExternalInput").ap()
    x = din("x", [NSEQ, S, D])
    w_in = din("w_in", [2, 1024, 2304])
    w_out = din("w_out", [2, 1024, 1024])
    gmlp_ln_g = din("gmlp_ln_g", [2, 256])
    gmlp_ln_b = din("gmlp_ln_b", [2, 256])
    gmlp_w_s = din("gmlp_w_s", [2, 4, 128, 128])
    gmlp_b_s = din("gmlp_b_s", [2, 4, 128])
    pool_w = din("pool_w", [2, 4, 64, 64])
    pool_scale = din("pool_scale", [2, 256])
    ln1_g = din("ln1_g", [2, 1024])
    ln1_b = din("ln1_b", [2, 1024])
    router_w = din("router_w", [1024, 16])
    router_bias = din("router_bias", [16])
    w_gate = din("w_gate", [2, 16, 1024, 512])
    w_up = din("w_up", [2, 16, 1024, 512])
    w_down = din("w_down", [2, 16, 512, 1024])
    ln2_g = din("ln2_g", [2, 1024])
    ln2_b = din("ln2_b", [2, 1024])
    c_masks = din("c_masks", [128, 1024], BF16)
    c_identb = din("c_identb", [128, 128], BF16)
    c_ident32 = din("c_ident32", [128, 128], F32)
    c_perm = din("c_perm", [128, 128], BF16)
    c_ropeC = din("c_ropeC", [128, S], BF16)
    c_ropeS = din("c_ropeS", [128, S], BF16)
    c_onesh = din("c_onesh", [128, 2, 128], BF16)
    c_rce = din("c_rce", [128, 2, 16], F32)
    out = dtn("out", [NSEQ, S, D], F32, kind="ExternalOutput").ap()
    scr = dtn("scr", [S, D], F32, kind="Internal").ap()
    dbg_out = None
    if dbg is not None:
        dbg_out = dtn("dbg", [128, 16384], F32, kind="ExternalOutput").ap()

    P = Plan()
    st = ExitStack()
    TOT = 204 * 1024
    arena = st.enter_context(nc.sbuf_tensor("arena", [128, TOT // 4], F32))
    banks = [st.enter_context(nc.psum_tensor("bank%d" % i, [128, 512], F32))[:] for i in range(8)]
    rbank = [Res("bank%d" % i) for i in range(8)]

    def f32v(off, n):
        assert off % 4 == 0
        return arena[:, off // 4: off // 4 + n]

    def bfv(off, n):
        assert off % 4 == 0 and n % 2 == 0
        return arena[:, off // 4: off // 4 + n // 2].bitcast(BF16)

    class Carve:
        def __init__(self, base, size):
            self.base, self.size, self.off = base, size, 0

        def reset(self):
            self.off = 0

        def f32(self, n):
            v = f32v(self.base + self.off, n)
            self.off += 4 * n
            assert self.off <= self.size, (self.off, self.size)
            return v

        def bf(self, n):
            n2 = (n + 1) // 2 * 2
            v = bfv(self.base + self.off, n2)
            self.off += 2 * n2
            assert self.off <= self.size, (self.off, self.size)
            return v[:, 0:n] if n2 != n else v

    o = 0
    XT_o = o; o += 32768
    X_o = o; o += 65536
    C_o = o; o += 32768
    D_o = o; o += 49152
    STG_o = o; o += 8192
    WT_o = o; o += 8192
    M_o = o
    misc = Carve(M_o, TOT - M_o)
    RX = Carve(X_o, 65536)
    RC = Carve(C_o, 32768)
    RD = Carve(D_o, 49152)

    xT = bfv(XT_o, 8 * S).rearrange("p (k t) -> p k t", k=8)
    r_xT = [[Res() for _ in range(NTB)] for _ in range(1)][0]
    stg = [f32v(STG_o + 2048 * i, 512) for i in range(4)]
    r_stg = [Res() for _ in range(4)]
    stg_i = [0]
    wt = [bfv(WT_o + 2048 * i, 1024).rearrange("p (k c) -> p k c", k=8) for i in range(4)]
    r_wt = [Res() for _ in range(4)]
    wt_i = [0]

    masks = misc.bf(1024)
    identb = misc.bf(128)
    ident32 = misc.f32(128)
    perm = misc.bf(128)
    onesh = misc.bf(256).rearrange("p (h c) -> p h c", h=2)
    rw32 = misc.f32(128).rearrange("p (k e) -> p k e", k=8)
    rbt = misc.f32(16)
    scores = misc.f32(256).rearrange("p (t e) -> p t e", t=16)
    gates = misc.f32(256).rearrange("p (t e) -> p t e", t=16)
    rt_b = misc.f32(256)
    rt_e = misc.f32(256)
    rt_b2 = misc.f32(256)
    rt_sel = misc.f32(256)
    rt_m1 = misc.f32(64)
    rt_m2 = misc.f32(64)
    rt_gs = misc.f32(64)
    rt_gm = misc.f32(16)
    rt_goh = misc.f32(64)
    rt_den = misc.f32(16)
    lnst = [misc.f32(12) for _ in range(2)]
    lnmv = [misc.f32(2) for _ in range(2)]
    lnrs = [misc.f32(1) for _ in range(2)]
    r_const = Res()
    r_scores = Res()
    r_gates = Res()
    r_rt = Res()
    r_ln = [Res(), Res()]

    def dma_in(dst, src, key, writes, reads=()):
        P.dma('sp', lambda e: e.dma_start(out=dst, in_=src), key, reads=reads, writes=writes)

    dma_in(masks, c_masks, 'd:c', [r_const])
    dma_in(identb, c_identb, 'd:c', [r_const])
    dma_in(ident32, c_ident32, 'd:c', [r_const])
    dma_in(perm, c_perm, 'd:c', [r_const])
    dma_in(onesh, c_onesh, 'd:c', [r_const])
    dma_in(rw32, router_w.rearrange("(k p) e -> p k e", p=128), 'd:c', [r_const])
    dma_in(rbt, router_bias.partition_broadcast(128), 'd:c', [r_const])

    def stage_cast(src, dst, r_dst_list, shape3=None):
        i = stg_i[0] % 4
        stg_i[0] += 1
        n = 1
        for d_ in dst.shape[1:]:
            n *= d_
        sv = stg[i][:, 0:n]
        if shape3 is not None:
            sv = sv.rearrange("p (a b) -> p a b", a=shape3)
        rs = r_stg[i]
        P.dma('sp', lambda e: e.dma_start(out=sv, in_=src), 'd:stg%d' % i, writes=[rs])
        P.op('act', lambda e: e.copy(out=dst, in_=sv), reads=[rs], writes=r_dst_list)

    def load_ftile(l, c0):
        i = wt_i[0] % 4
        wt_i[0] += 1
        wv = w_in[l].rearrange("(k p) c -> p k c", p=128)
        for h in range(2):
            stage_cast(wv[:, 4 * h:4 * h + 4, c0:c0 + 128], wt[i][:, 4 * h:4 * h + 4, :], [r_wt[i]], shape3=4)
        return wt[i], r_wt[i]

    def tok_cols(tb):
        return slice(tb * 128, (tb + 1) * 128)

    def ln_stats(xt_ap, r_x, slot):
        stt, mv, rs = lnst[slot], lnmv[slot], lnrs[slot]
        rl = r_ln[slot]
        P.op('dve', lambda e: e.bn_stats(out=stt[:, 0:6], in_=xt_ap[:, 0:512]), reads=[r_x], writes=[rl])
        P.op('dve', lambda e: e.bn_stats(out=stt[:, 6:12], in_=xt_ap[:, 512:1024]), reads=[r_x], writes=[rl])
        P.op('dve', lambda e: e.bn_aggr(out=mv, in_=stt), reads=[rl], writes=[rl])
        P.op('act', lambda e: e.activation(out=rs, in_=mv[:, 1:2], func=AF.Ln, bias=EPS), reads=[rl], writes=[rl])
        P.op('act', lambda e: e.activation(out=rs, in_=rs, func=AF.Exp, scale=-0.5), reads=[rl], writes=[rl])

    def ln_apply(xt_ap, r_x, gtile, btile, r_gb, slot):
        mv, rs = lnmv[slot], lnrs[slot]
        rl = r_ln[slot]
        P.op('dve', lambda e: e.tensor_scalar(out=xt_ap, in0=xt_ap, scalar1=mv[:, 0:1], scalar2=rs[:, 0:1],
                                              op0=ALU.subtract, op1=ALU.mult), reads=[r_x, rl], writes=[r_x])
        P.op('dve', lambda e: e.tensor_mul(out=xt_ap, in0=xt_ap, in1=gtile), reads=[r_x, r_gb], writes=[r_x])
        P.op('dve', lambda e: e.tensor_add(out=xt_ap, in0=xt_ap, in1=btile), reads=[r_x, r_gb], writes=[r_x])

    def to_xT_bf16(src_f32, r_src, ybf, r_ybf, tb, bank):
        P.op('act', lambda e: e.copy(out=ybf, in_=src_f32), reads=[r_src], writes=[r_ybf])
        pb = banks[bank][:, 0:512].bitcast(BF16)
        for k in range(8):
            P.op('pe', lambda e, k=k: e.transpose(out=pb[:, k * 128:(k + 1) * 128], in_=ybf[:, k * 128:(k + 1) * 128],
                                                  identity=identb), reads=[r_ybf, r_const], writes=[rbank[bank]])
        P.op('act', lambda e: e.copy(out=xT[:, :, tok_cols(tb)], in_=pb.rearrange("p (k c) -> p k c", k=8)),
             reads=[rbank[bank]], writes=[r_xT[tb]])

    dump_i = [0]

    def dump(ap_f32_2d, reads):
        n = ap_f32_2d.shape[1]
        o0 = dump_i[0]
        dump_i[0] += n
        rr = Res()
        P.dma('sp', lambda e: e.dma_start(out=dbg_out[:, o0:o0 + n], in_=ap_f32_2d), 'd:dbg', reads=reads, writes=[rr])
        return rr

    r_outs = []

    try:
      for s in range(NSEQ):
          P.barrier()
          RD.reset()
          xin = [RD.f32(1024) for _ in range(2)]
          r_xin = [Res(), Res()]
          ybf = [RD.bf(1024) for _ in range(2)]
          r_ybf = [Res(), Res()]
          for tb in range(NTB):
              sl = tb % 2
              dma_in(xin[sl], x[s, tb * 128:(tb + 1) * 128, :], 'd:xin%d' % sl, [r_xin[sl]])
              to_xT_bf16(xin[sl], r_xin[sl], ybf[sl], r_ybf[sl], tb, 6 + sl)

          for l in range(DEPTH):
              x_src = x[s] if l == 0 else scr
              last = (l == DEPTH - 1)
              P.barrier()
              RX.reset(); RC.reset(); RD.reset()
              catT = RC.bf(8 * S).rearrange("p (k t) -> p k t", k=8)
              r_cat = [[Res() for _ in range(4)] for _ in range(8)]
              uT = RX.bf(2 * S).rearrange("p (k t) -> p k t", k=2)
              r_uT = [[Res() for _ in range(4)] for _ in range(2)]
              wvg = RX.bf(8 * 256).rearrange("p (k c) -> p k c", k=8)
              r_wvg = Res()
              ws32 = RX.f32(512).rearrange("p (g c) -> p g c", g=4)
              wsn = RX.bf(512).rearrange("p (g c) -> p g c", g=4)
              wsT = RX.bf(512).rearrange("p (g c) -> p g c", g=4)
              r_ws = Res()
              bsb = RX.f32(256).rearrange("p (t c) -> p t c", t=2)
              lng = RX.f32(256)
              lnb = RX.f32(256)
              r_gp = Res()
              wpb32 = RX.f32(256).rearrange("p (t c) -> p t c", t=2)
              wpbd = RX.bf(256).rearrange("p (t c) -> p t c", t=2)
              r_wpb = Res()
              psc = RX.f32(2)
              rce = RX.f32(32).rearrange("p (t c) -> p t c", t=2)
              gv = [RX.f32(512) for _ in range(2)]
              g2 = [RX.f32(512) for _ in range(2)]
              xc = [RX.f32(512) for _ in range(2)]
              vnp = [RX.bf(1024).rearrange("p (j t e c) -> p j t e c", j=2, t=2, e=2) for _ in range(2)]
              r_g = [Res(), Res()]
              r_g2 = [Res(), Res()]
              r_vnp = [Res(), Res()]
              sm = [[RX.f32(8) for _ in range(6)] for _ in range(2)]
              gtmp = [RX.f32(512) for _ in range(2)]
              r_gtmp = [Res(), Res()]
              etmp = RX.f32(8)
              pzp = RD.f32(2 * 2080).rearrange("p (t c) -> p t c", t=2)
              r_pzp = [Res(), Res()]
              sA = RD.f32(2080)
              sB = RD.f32(2080)
              r_sA, r_sB = Res(), Res()
              pooled = RD.bf(2 * S).rearrange("p (t c) -> p t c", t=2)
              r_pooled = [Res(), Res()]

              pre_ft = {}
              for c0_ in (1536, 1664, 2048, 2176):
                  pre_ft[c0_] = load_ftile(l, c0_)
              wv_ = w_in[l].rearrange("(k p) c -> p k c", p=128)
              for k2 in range(4):
                  stage_cast(wv_[:, 2 * k2:2 * k2 + 2, 1792:2048], wvg[:, 2 * k2:2 * k2 + 2, :], [r_wvg], shape3=2)
              for g in range(4):
                  dma_in(ws32[:, g, :], gmlp_w_s[l, g], 'd:gp', [r_ws])
              for t in range(2):
                  for e_ in range(2):
                      dma_in(bsb[e_ * 64:(e_ + 1) * 64, t, :], gmlp_b_s[l, 2 * t + e_].partition_broadcast(64), 'd:gp', [r_gp])
              dma_in(lng, gmlp_ln_g[l].partition_broadcast(128), 'd:gp', [r_gp])
              dma_in(lnb, gmlp_ln_b[l].partition_broadcast(128), 'd:gp', [r_gp])
              P.op('pool', lambda e: e.memset(wpb32, 0.0), writes=[r_wpb])
              for t in range(2):
                  for e_ in range(2):
                      dma_in(wpb32[e_ * 64:(e_ + 1) * 64, t, e_ * 64:(e_ + 1) * 64], pool_w[l, 2 * t + e_], 'd:gp', [r_wpb])
                  dma_in(psc[:, t:t + 1], pool_scale[l, t * 128:(t + 1) * 128].rearrange("(p o) -> p o", o=1), 'd:gp', [r_gp])
              dma_in(rce, c_rce, 'd:gp', [r_gp])
              P.op('pool', lambda e: e.tensor_copy(out=wpbd, in_=wpb32), reads=[r_wpb], writes=[r_wpb])
              P.op('pool', lambda e: e.tensor_copy(out=wsn, in_=ws32), reads=[r_ws], writes=[r_ws])
              wsp = banks[7][:, 0:256].bitcast(BF16)
              for g in range(4):
                  P.op('pe', lambda e, g=g: e.transpose(out=wsp[:, g * 128:(g + 1) * 128], in_=wsn[:, g, :], identity=identb),
                       reads=[r_ws, r_const], writes=[rbank[7]])
              P.op('act', lambda e: e.copy(out=wsT, in_=wsp.rearrange("p (g c) -> p g c", g=4)), reads=[rbank[7]], writes=[r_ws])
              for vi in range(2):
                  P.op('pool', lambda e, vi=vi: e.memset(vnp[vi], 0.0), writes=[r_vnp[vi]])
              P.op('pool', lambda e: e.memset(pzp, 0.0), writes=r_pzp)

              pbi = [0]

              def proj_ftile(c0, epilogue):
                  wtile, rw_ = pre_ft[c0]
                  for tt in range(4):
                      b = pbi[0] % 2
                      pbi[0] += 1
                      cols = slice(tt * 512, (tt + 1) * 512)
                      for k in range(8):
                          P.op('pe', lambda e, k=k, b=b, cols=cols: e.matmul(banks[b], lhsT=wtile[:, k, :], rhs=xT[:, k, cols],
                                                                             start=(k == 0), stop=(k == 7)),
                               reads=[rw_] + r_xT[tt * 4:(tt + 1) * 4], writes=[rbank[b]])
                      epilogue(tt, b, cols)

              for t in range(2):
                  def ep_u(tt, b, cols, t=t):
                      P.op('act', lambda e: e.activation(out=uT[:, t, cols], in_=banks[b], func=AF.Gelu_apprx_tanh),
                           reads=[rbank[b]], writes=[r_uT[t][tt]])
                  proj_ftile(1536 + t * 128, ep_u)
              pz_eps = {}
              for t in range(2):
                  def ep_p(tt, b, cols, t=t):
                      P.op('act', lambda e: e.copy(out=pzp[:, t, 16 + tt * 512:16 + (tt + 1) * 512], in_=banks[b]),
                           reads=[rbank[b]], writes=[r_pzp[t]])
                  pz_eps[t] = ep_p

              def pool_part(t):
                  Z = pzp[:, t, :]
                  P.op('dve', lambda e, Z=Z: e.tensor_add(out=sA[:, 1:2080], in0=Z[:, 1:2080], in1=Z[:, 0:2079]),
                       reads=[r_pzp[t]], writes=[r_sA])
                  P.op('dve', lambda e: e.tensor_add(out=sB[:, 2:2079], in0=sA[:, 3:2080], in1=sA[:, 1:2078]),
                       reads=[r_sA], writes=[r_sB])
                  if t == 1:
                      P.op('dve', lambda e: e.tensor_add(out=sA[:, 4:2077], in0=sB[:, 2:2075], in1=sB[:, 6:2079]),
                           reads=[r_sB], writes=[r_sA])
                      P.op('dve', lambda e: e.tensor_add(out=sB[:, 8:2073], in0=sA[:, 4:2069], in1=sA[:, 12:2077]),
                           reads=[r_sA], writes=[r_sB])
                  for e_ in range(2):
                      w_ = {(0, 0): 2, (1, 0): 4, (0, 1): 8, (1, 1): 16}[(e_, t)]
                      sb_ = sA if e_ == 0 else sB
                      rows = slice(e_ * 64, (e_ + 1) * 64)
                      P.op('dve', lambda e, sb_=sb_, rows=rows, w_=w_, Z=Z, t=t: e.scalar_tensor_tensor(
                          out=pooled[rows, t, :], in0=sb_[rows, 16:16 + S], scalar=1.0 / w_, in1=Z[rows, 16:16 + S],
                          op0=ALU.mult, op1=ALU.subtract), reads=[r_sA, r_sB, r_pzp[t]], writes=[r_pooled[t]])
                      for (c0, r0) in ((0, 0), (S - 8, 8)):
                          P.op('pool', lambda e, sb_=sb_, rows=rows, c0=c0, r0=r0, t=t: e.tensor_mul(
                              out=etmp[rows, :], in0=sb_[rows, 16 + c0:16 + c0 + 8], in1=rce[rows, t, r0:r0 + 8]),
                              reads=[r_sA, r_sB, r_gp], writes=[r_gtmp[0]])
                          P.op('pool', lambda e, rows=rows, c0=c0, Z=Z, t=t: e.tensor_sub(
                              out=pooled[rows, t, c0:c0 + 8], in0=etmp[rows, :], in1=Z[rows, 16 + c0:16 + c0 + 8]),
                              reads=[r_gtmp[0], r_pzp[t]], writes=[r_pooled[t]])
                  for tt in range(4):
                      b = pbi[0] % 2
                      pbi[0] += 1
                      cols = slice(tt * 512, (tt + 1) * 512)
                      P.op('pe', lambda e, b=b, cols=cols, t=t: e.matmul(banks[b], lhsT=wpbd[:, t, :], rhs=pooled[:, t, cols],
                                                                         start=True, stop=True),
                           reads=[r_wpb, r_pooled[t]], writes=[rbank[b]])
                      P.op('act', lambda e, b=b, cols=cols, t=t: e.activation(out=catT[:, 6 + t, cols], in_=banks[b], func=AF.Copy,
                                                                              scale=psc[:, t:t + 1]),
                           reads=[rbank[b], r_gp], writes=[r_cat[6 + t][tt]])

              def gm_stage_a(p):
                  vi = p % 2
                  pb = 2 + vi
                  for jj in range(2):
                      tb = 2 * p + jj
                      for k in range(8):
                          P.op('pe', lambda e, k=k, pb=pb, tb=tb, jj=jj: e.matmul(banks[pb][:, jj * 256:(jj + 1) * 256], lhsT=xT[:, k, tok_cols(tb)],
                                                                                 rhs=wvg[:, k, :], start=(k == 0), stop=(k == 7)),
                               reads=[r_xT[tb], r_wvg], writes=[rbank[pb]])
                  G, G2, XC = gv[vi], g2[vi], xc[vi]
                  s1, s2, mean, msq, var, rstd = sm[vi]
                  rg = r_g[vi]
                  rs_ = r_g2[vi]
                  P.op('act', lambda e, G=G, pb=pb: e.activation(out=G, in_=banks[pb], func=AF.Gelu_apprx_tanh),
                       reads=[rbank[pb]], writes=[rg])
                  P.op('act', lambda e, G=G, G2=G2: e.activation(out=G2, in_=G, func=AF.Square), reads=[rg], writes=[rs_])
                  G3 = G.rearrange("p (g c) -> p g c", g=8)
                  G23 = G2.rearrange("p (g c) -> p g c", g=8)
                  P.op('dve', lambda e, s1=s1, G3=G3: e.reduce_sum(out=s1, in_=G3, axis=AX.X), reads=[rg], writes=[rs_])
                  P.op('dve', lambda e, s2=s2, G23=G23: e.reduce_sum(out=s2, in_=G23, axis=AX.X), reads=[rs_], writes=[rs_])
                  P.op('dve', lambda e, mean=mean, s1=s1: e.tensor_scalar_mul(out=mean, in0=s1, scalar1=1.0 / 64), reads=[rs_], writes=[rs_])
                  P.op('dve', lambda e, msq=msq, mean=mean: e.tensor_mul(out=msq, in0=mean, in1=mean), reads=[rs_], writes=[rs_])
                  P.op('dve', lambda e, var=var, s2=s2, msq=msq: e.scalar_tensor_tensor(out=var, in0=s2, scalar=1.0 / 64, in1=msq,
                                                                                       op0=ALU.mult, op1=ALU.subtract), reads=[rs_], writes=[rs_])
                  P.op('act', lambda e, rstd=rstd, var=var: e.activation(out=rstd, in_=var, func=AF.Sqrt, bias=EPS), reads=[rs_], writes=[rs_])
                  P.op('dve', lambda e, rstd=rstd: e.reciprocal(out=rstd, in_=rstd), reads=[rs_], writes=[rs_])

              def gm_stage_b(p):
                  vi = p % 2
                  G, G2, XC = gv[vi], g2[vi], xc[vi]
                  s1, s2, mean, msq, var, rstd = sm[vi]
                  rg = r_g[vi]
                  rs_ = r_g2[vi]
                  G3 = G.rearrange("p (g c) -> p g c", g=8)
                  XC3 = XC.rearrange("p (g c) -> p g c", g=8)
                  P.op('dve', lambda e, XC3=XC3, G3=G3, mean=mean: e.tensor_sub(
                      out=XC3, in0=G3, in1=mean.unsqueeze(2).to_broadcast([128, 8, 64])), reads=[rg, rs_], writes=[rg])
                  P.op('dve', lambda e, XC3=XC3, rstd=rstd: e.tensor_mul(
                      out=XC3, in0=XC3, in1=rstd.unsqueeze(2).to_broadcast([128, 8, 64])), reads=[rg, rs_], writes=[rg])
                  XCj = XC.rearrange("p (j c) -> p j c", j=2)
                  P.op('pool', lambda e, XCj=XCj: e.tensor_mul(out=XCj, in0=XCj, in1=lng.unsqueeze(1).to_broadcast([128, 2, 256])),
                       reads=[rg, r_gp], writes=[rg])
                  XC5 = XC.rearrange("p (j t e c) -> p j t e c", j=2, t=2, e=2)
                  LB4 = lnb.rearrange("p (t e c) -> p t e c", t=2, e=2)
                  for e_ in range(2):
                      P.op('pool', lambda e, e_=e_, vi=vi, XC5=XC5: e.tensor_add(
                          out=vnp[vi][:, :, :, e_, e_ * 64:(e_ + 1) * 64], in0=XC5[:, :, :, e_, :],
                          in1=LB4[:, :, e_, :].unsqueeze(1).to_broadcast([128, 2, 2, 64])),
                          reads=[rg, r_gp], writes=[r_vnp[vi]])
                  grp = p // 2
                  for jj in range(2):
                      j = (p % 2) * 2 + jj
                      for t in range(2):
                          sb_ = 4 + t
                          for e_ in range(2):
                              P.op('pe', lambda e, t=t, e_=e_, vi=vi, sb_=sb_, j=j, jj=jj: e.matmul(
                                  banks[sb_][:, j * 128:(j + 1) * 128], lhsT=vnp[vi][:, jj, t, e_, :], rhs=wsT[:, 2 * t + e_, :],
                                  start=(e_ == 0), stop=(e_ == 1)), reads=[r_vnp[vi], r_ws], writes=[rbank[sb_]])
                  if p % 2 == 1:
                      cols = slice(grp * 512, (grp + 1) * 512)
                      for t in range(2):
                          sb_ = 4 + t
                          P.op('dve', lambda e, t=t, sb_=sb_: e.tensor_add(
                              out=gtmp[t].rearrange("p (j c) -> p j c", j=4), in0=banks[sb_].rearrange("p (j c) -> p j c", j=4),
                              in1=bsb[:, t, :].unsqueeze(1).to_broadcast([128, 4, 128])), reads=[rbank[sb_], r_gp], writes=[r_gtmp[t]])
                          P.op('pool', lambda e, t=t, cols=cols: e.tensor_mul(out=catT[:, 4 + t, cols], in0=gtmp[t], in1=uT[:, t, cols]),
                               reads=[r_gtmp[t], r_uT[t][grp]], writes=[r_cat[4 + t][grp]])

              gm_stage_a(0)
              extra = {0: lambda: proj_ftile(2048, pz_eps[0]), 1: lambda: proj_ftile(2176, pz_eps[1]),
                       2: lambda: pool_part(0), 4: lambda: pool_part(1)}
              for p in range(8):
                  if p in extra:
                      extra[p]()
                  if p + 1 < 8:
                      gm_stage_a(p + 1)
                  gm_stage_b(p)

              if dbg == 'm4':
                  raise StopBuild()

              P.barrier()
              RX.reset(); RD.reset()
              qz = RX.bf(2 * S).rearrange("p (h t) -> p h t", h=2)
              kT_ = RX.bf(S)
              r_qk = [[Res() for _ in range(4)] for _ in range(2)]
              vpad = [RX.bf(16 * 2 * 128).rearrange("p (b h c) -> p b h c", b=16, h=2) for _ in range(2)]
              r_vpad = [Res(), Res()]
              acc = RX.f32(2 * S).rearrange("p (a t) -> p a t", a=2)
              r_acc = Res()
              PT = [RX.bf(512) for _ in range(4)]
              r_PT = [Res() for _ in range(4)]
              qraw = [RD.bf(512) for _ in range(2)]
              r_qraw = [Res(), Res()]
              t1 = [RD.f32(512) for _ in range(2)]
              t2 = [RD.f32(512) for _ in range(2)]
              r_t = [Res(), Res()]
              ropeC = RD.bf(S)
              ropeS = RD.bf(S)
              r_rope = Res()
              dma_in(ropeC, c_ropeC, 'd:gp', [r_rope])
              dma_in(ropeS, c_ropeS, 'd:gp', [r_rope])
              for vi in range(2):
                  P.op('pool', lambda e, vi=vi: e.memset(vpad[vi], 0.0), writes=[r_vpad[vi]])
              P.op('pool', lambda e: e.memset(qz, 0.0), writes=r_qk[0])
              vslot = [0]
              ui = [0]
              nli = [0]
              for hp in range(4):
                  for qi_ in range(2):
                      wtile, rw_ = load_ftile(l, qi_ * 512 + hp * 128)
                      for tt in range(4):
                          sl = (qi_ * 4 + tt) % 2
                          cols = slice(tt * 512, (tt + 1) * 512)
                          pq = (qi_ * 4 + tt) % 4
                          pp = 6 + sl
                          for k in range(8):
                              P.op('pe', lambda e, k=k, cols=cols, wtile=wtile, pq=pq: e.matmul(banks[pq], lhsT=wtile[:, k, :], rhs=xT[:, k, cols],
                                                                                               start=(k == 0), stop=(k == 7)),
                                   reads=[rw_] + r_xT[tt * 4:(tt + 1) * 4], writes=[rbank[pq]])
                          P.op('act', lambda e, sl=sl, pq=pq: e.copy(out=qraw[sl], in_=banks[pq]), reads=[rbank[pq]], writes=[r_qraw[sl]])
                          P.op('dve', lambda e, sl=sl, cols=cols: e.tensor_mul(out=t1[sl], in0=qraw[sl], in1=ropeC[:, cols]),
                               reads=[r_qraw[sl], r_rope], writes=[r_t[sl]])
                          P.op('pe', lambda e, sl=sl, pp=pp: e.matmul(banks[pp], lhsT=perm, rhs=qraw[sl], start=True, stop=True),
                               reads=[r_qraw[sl], r_const], writes=[rbank[pp]])
                          P.op('dve', lambda e, sl=sl, cols=cols, pp=pp: e.tensor_mul(out=t2[sl], in0=banks[pp], in1=ropeS[:, cols]),
                               reads=[rbank[pp], r_rope], writes=[r_t[sl]])
                          if qi_ == 1:
                              P.op('dve', lambda e, sl=sl, cols=cols: e.tensor_add(out=kT_[:, cols], in0=t1[sl], in1=t2[sl]),
                                   reads=[r_t[sl]], writes=[r_qk[1][tt]])
                          else:
                              for hh in range(2):
                                  rows = slice(hh * 64, (hh + 1) * 64)
                                  P.op('dve', lambda e, sl=sl, cols=cols, hh=hh, rows=rows: e.tensor_add(
                                      out=qz[rows, hh, cols], in0=t1[sl][rows, :], in1=t2[sl][rows, :]),
                                       reads=[r_t[sl]], writes=[r_qk[0][tt]])
                  if dbg == 'm3a':
                      raise StopBuild()
                  wv, r_wv = load_ftile(l, 1024 + hp * 128)
                  P.op('pool', lambda e: e.memset(acc, 0.0), writes=[r_acc])
                  def do_cfg_v(dil, L):
                      vs = vslot[0] % 2
                      vslot[0] += 1
                      VP = vpad[vs]
                      nbr = L // 128
                      for g4 in range(4):
                          for j in range(4):
                              blk = g4 * 4 + j
                              r_, jb = blk // nbr, blk % nbr
                              t0 = r_ + dil * jb * 128
                              tsl = slice(t0, t0 + dil * 127 + 1, dil)
                              rd = r_xT if dil > 1 else [r_xT[blk]]
                              for k in range(8):
                                  P.op('pe', lambda e, k=k, tsl=tsl, j=j, wv=wv, vb=6 + g4 % 2: e.matmul(banks[vb][:, j * 128:(j + 1) * 128], lhsT=xT[:, k, tsl],
                                                                                 rhs=wv[:, k, :], start=(k == 0), stop=(k == 7)),
                                       reads=[r_wv] + rd, writes=[rbank[6 + g4 % 2]])
                          b6 = banks[6 + g4 % 2].rearrange("p (j h c) -> p j h c", j=4, h=2)
                          for h in range(2):
                              P.op('act', lambda e, g4=g4, h=h, VP=VP, b6=b6: e.copy(
                                  out=VP[:, g4 * 4:(g4 + 1) * 4, h, h * 64:(h + 1) * 64], in_=b6[:, :, h, :]),
                                  reads=[rbank[6 + g4 % 2]], writes=[r_vpad[vs]])
                      return VP, vs, nbr

                  def do_cfg_units(dil, L, VP, vs, nbr):
                      allunits = []
                      for r_ in range(dil):
                          if L == 128:
                              units = [(0, 128, [(0, 768)])]
                          else:
                              units = [(0, 64, [(0, 512)])]
                              for i in range(L // 128 - 1):
                                  units.append((128 * i + 64, 128, [(i, 0), (i + 1, 128)]))
                              units.append((L - 64, 64, [(L // 128 - 1, 640)]))
                          for (qa, nq, kbs) in units:
                              allunits.append((r_, qa, nq, kbs))

                      def rcols(r_, a, n, dil=dil):
                          t0 = r_ + dil * a
                          return slice(t0, t0 + dil * (n - 1) + 1, dil)

                      def stage_s(un):
                          r_, qa, nq, kbs = un
                          u_ = ui[0]
                          ui[0] += 1
                          sbk = u_ % 4
                          nk = len(kbs)
                          tot = 2 * nk * nq
                          m0 = kbs[0][1]
                          mbase = 0 if nk == 2 else m0
                          qc_ = rcols(r_, qa, nq)
                          for hh in range(2):
                              for ki_, (kb, _) in enumerate(kbs):
                                  off = (hh * nk + ki_) * nq
                                  kc_ = rcols(r_, kb * 128, 128)
                                  P.op('pe', lambda e, hh=hh, kc_=kc_, qc_=qc_, off=off, nq=nq, sbk=sbk: e.matmul(
                                      banks[sbk][:, off:off + nq], lhsT=kT_[:, kc_], rhs=qz[:, hh, qc_],
                                      start=True, stop=True), reads=r_qk[0] + r_qk[1], writes=[rbank[sbk]])
                          P.op('act', lambda e, sbk=sbk, tot=tot: e.activation(out=PT[sbk][:, 0:tot], in_=banks[sbk][:, 0:tot],
                                                                              func=AF.Exp, scale=0.125),
                               reads=[rbank[sbk]], writes=[r_PT[sbk]])
                          P.op('pool' if u_ % 2 == 0 else 'dve', lambda e, sbk=sbk, tot=tot, mbase=mbase: e.tensor_mul(
                              out=PT[sbk][:, 0:tot], in0=PT[sbk][:, 0:tot], in1=masks[:, mbase:mbase + tot]),
                               reads=[r_PT[sbk], r_const], writes=[r_PT[sbk]])
                          return sbk

                      pvst = {'col': 0, 'first': 0, 'nbk': 4}

                      def stage_pv(un, sbk, VP=VP, vs=vs, nbr=nbr, L=L):
                          r_, qa, nq, kbs = un
                          nk = len(kbs)
                          if pvst['col'] == 0:
                              nli[0] += 1
                              pvst['nbk'] = 4 + nli[0] % 2
                              pvst['first'] = qa
                          nbk = pvst['nbk']
                          col = pvst['col']
                          n_mm = 2 * nk
                          i_mm = 0
                          for hh in range(2):
                              for ki_, (kb, _) in enumerate(kbs):
                                  off = (hh * nk + ki_) * nq
                                  blk = r_ * nbr + kb
                                  P.op('pe', lambda e, hh=hh, blk=blk, off=off, nq=nq, col=col, i_mm=i_mm, n_mm=n_mm, nbk=nbk, sbk=sbk: e.matmul(
                                      banks[nbk][:, col:col + nq], lhsT=VP[:, blk, hh, :], rhs=PT[sbk][:, off:off + nq],
                                      start=(i_mm == 0), stop=(i_mm == n_mm - 1)), reads=[r_PT[sbk], r_vpad[vs]], writes=[rbank[nbk]])
                                  i_mm += 1
                          i_mm = 0
                          for hh in range(2):
                              for ki_, (kb, _) in enumerate(kbs):
                                  off = (hh * nk + ki_) * nq
                                  P.op('pe', lambda e, hh=hh, off=off, nq=nq, col=col, i_mm=i_mm, n_mm=n_mm, nbk=nbk, sbk=sbk: e.matmul(
                                      banks[nbk][:, 256 + col:256 + col + nq], lhsT=onesh[:, hh, :], rhs=PT[sbk][:, off:off + nq],
                                      start=(i_mm == 0), stop=(i_mm == n_mm - 1)), reads=[r_PT[sbk], r_const], writes=[rbank[nbk]])
                                  i_mm += 1
                          col += nq
                          is_last = (qa + nq == L)
                          if col + 128 > 256 or is_last:
                              acols = rcols(r_, pvst['first'], col)
                              P.op('dve', lambda e, acols=acols, col=col, nbk=nbk: e.tensor_add(
                                  out=acc[:, :, acols], in0=acc[:, :, acols],
                                  in1=banks[nbk].rearrange("p (a c) -> p a c", a=2)[:, :, 0:col]),
                                   reads=[rbank[nbk], r_acc], writes=[r_acc])
                              col = 0
                          pvst['col'] = col

                      LA = 3
                      sb_of = {}
                      for i in range(len(allunits) + LA):
                          if i < len(allunits):
                              sb_of[i] = stage_s(allunits[i])
                          if i - LA >= 0:
                              stage_pv(allunits[i - LA], sb_of[i - LA])
                      if dbg == 'm3b':
                          raise StopBuild()

                  cfgs = ((1, 2048), (4, 512), (16, 128))
                  vinfo = do_cfg_v(*cfgs[0])
                  for ci in range(3):
                      nxt = do_cfg_v(*cfgs[ci + 1]) if ci + 1 < 3 else None
                      do_cfg_units(cfgs[ci][0], cfgs[ci][1], *vinfo)
                      vinfo = nxt
                  P.op('act', lambda e: e.activation(out=acc[:, 1, :], in_=acc[:, 1, :], func=AF.Ln), reads=[r_acc], writes=[r_acc])
                  P.op('act', lambda e: e.activation(out=acc[:, 1, :], in_=acc[:, 1, :], func=AF.Exp, scale=-1.0), reads=[r_acc], writes=[r_acc])
                  P.op('dve', lambda e, hp=hp: e.tensor_mul(out=catT[:, hp, :], in0=acc[:, 0, :], in1=acc[:, 1, :]), reads=[r_acc], writes=r_cat[hp])

              if dbg == 'm3':
                  raise StopBuild()

              P.barrier()
              RX.reset(); RD.reset()
              x1 = RX.f32(NTB * D).rearrange("p (t c) -> p t c", t=NTB)
              r_x1 = [Res() for _ in range(NTB)]
              wout = RD.bf(8 * 1024).rearrange("p (k c) -> p k c", k=8)
              r_wout = Res()
              xin = [RD.f32(1024) for _ in range(2)]
              r_xin = [Res(), Res()]
              xr32 = [RD.f32(1024).rearrange("p (k c) -> p k c", k=8) for _ in range(2)]
              r_xr = [Res(), Res()]
              g1b = RD.f32(1024)
              b1b = RD.f32(1024)
              r_g1 = Res()
              dma_in(g1b, ln1_g[l].partition_broadcast(128), 'd:gp', [r_g1])
              dma_in(b1b, ln1_b[l].partition_broadcast(128), 'd:gp', [r_g1])
              wo_ = w_out[l].rearrange("(k p) c -> p k c", p=128)
              for k in range(8):
                  for h in range(2):
                      stage_cast(wo_[:, k, h * 512:(h + 1) * 512], wout[:, k, h * 512:(h + 1) * 512], [r_wout])
              def m5_a(tb):
                  sl = tb % 2
                  dma_in(xin[sl], x_src[tb * 128:(tb + 1) * 128, :], 'd:xin%d' % sl, [r_xin[sl]],
                         reads=[r_scr] if l > 0 else ())
                  for h in range(2):
                      ob = 2 * sl + h
                      for k in range(8):
                          P.op('pe', lambda e, k=k, h=h, ob=ob, tb=tb: e.matmul(banks[ob], lhsT=catT[:, k, tok_cols(tb)],
                                                                                rhs=wout[:, k, h * 512:(h + 1) * 512],
                                                                                start=(k == 0), stop=(k == 7)),
                               reads=[r_cat[k][tb // 4], r_wout], writes=[rbank[ob]])
                      P.op('dve', lambda e, h=h, ob=ob, tb=tb, sl=sl: e.scalar_tensor_tensor(
                          out=x1[:, tb, h * 512:(h + 1) * 512], in0=xin[sl][:, h * 512:(h + 1) * 512], scalar=ALPHA,
                          in1=banks[ob], op0=ALU.mult, op1=ALU.add), reads=[r_xin[sl], rbank[ob]], writes=[r_x1[tb]])

              def m5_a2(tb):
                  ln_stats(x1[:, tb, :], r_x1[tb], tb % 2)

              def m5_b1(tb):
                  sl = tb % 2
                  ln_apply(x1[:, tb, :], r_x1[tb], g1b, b1b, r_g1, sl)
                  for h in range(2):
                      tbk = 4 + h
                      for k4 in range(4):
                          k = h * 4 + k4
                          P.op('pe', lambda e, k=k, k4=k4, tbk=tbk, tb=tb: e.transpose(
                              out=banks[tbk][:, k4 * 128:(k4 + 1) * 128], in_=x1[:, tb, k * 128:(k + 1) * 128], identity=ident32),
                              reads=[r_x1[tb], r_const], writes=[rbank[tbk]])
                      b3 = banks[tbk].rearrange("p (k c) -> p k c", k=4)
                      P.op('dve', lambda e, h=h, sl=sl, b3=b3: e.tensor_copy(out=xr32[sl][:, h * 4:(h + 1) * 4, :], in_=b3),
                           reads=[rbank[tbk]], writes=[r_xr[sl]])
                      P.op('act', lambda e, h=h, tb=tb, sl=sl: e.copy(out=xT[:, h * 4:(h + 1) * 4, tok_cols(tb)], in_=xr32[sl][:, h * 4:(h + 1) * 4, :]),
                           reads=[r_xr[sl]], writes=[r_xT[tb]])

              def m5_b2(tb):
                  sl = tb % 2
                  for k in range(8):
                      P.op('pe', lambda e, k=k, sl=sl: e.matmul(banks[6][:, 0:16], lhsT=xr32[sl][:, k, :], rhs=rw32[:, k, :],
                                                                start=(k == 0), stop=(k == 7)),
                           reads=[r_xr[sl], r_const], writes=[rbank[6]])
                  P.op('act', lambda e, tb=tb: e.activation(out=scores[:, tb, :], in_=banks[6][:, 0:16], func=AF.Exp, scale=-1.0),
                       reads=[rbank[6]], writes=[r_scores])
                  P.op('act', lambda e, tb=tb: e.mul(out=x1[:, tb, :], in_=x1[:, tb, :], mul=ALPHA),
                       reads=[r_x1[tb]], writes=[r_x1[tb]])

              for i in range(-3, NTB):
                  if 0 <= i + 3 < NTB:
                      m5_a(i + 3)
                  if 0 <= i + 2 < NTB:
                      m5_a2(i + 2)
                  if 0 <= i + 1 < NTB:
                      m5_b1(i + 1)
                  if 0 <= i:
                      m5_b2(i)

              sc2 = scores.rearrange("p t e -> p (t e)")
              P.op('dve', lambda e: e.tensor_scalar_add(out=sc2, in0=sc2, scalar1=1.0), reads=[r_scores], writes=[r_scores])
              P.op('dve', lambda e: e.reciprocal(out=sc2, in_=sc2), reads=[r_scores], writes=[r_scores])
              B3 = rt_b.rearrange("p (a c) -> p a c", c=4)
              E3 = rt_e.rearrange("p (a c) -> p a c", c=4)
              B23 = rt_b2.rearrange("p (a c) -> p a c", c=4)
              SL3 = rt_sel.rearrange("p (a c) -> p a c", c=4)
              rr = [r_rt]
              P.op('dve', lambda e: e.tensor_add(out=rt_b.rearrange("p (t e) -> p t e", t=16), in0=scores,
                                                 in1=rbt.unsqueeze(1).to_broadcast([128, 16, 16])), reads=[r_scores, r_const], writes=rr)
              P.op('dve', lambda e: e.reduce_max(out=rt_m1, in_=B3, axis=AX.X), reads=rr, writes=rr)
              P.op('dve', lambda e: e.tensor_tensor(out=E3, in0=B3, in1=rt_m1.unsqueeze(2).to_broadcast([128, 64, 4]), op=ALU.is_equal),
                   reads=rr, writes=rr)
              P.op('dve', lambda e: e.scalar_tensor_tensor(out=rt_b2, in0=rt_e, scalar=-1e9, in1=rt_b, op0=ALU.mult, op1=ALU.add),
                   reads=rr, writes=rr)
              P.op('dve', lambda e: e.reduce_max(out=rt_m2, in_=B23, axis=AX.X), reads=rr, writes=rr)
              P.op('dve', lambda e: e.tensor_add(out=rt_gs, in0=rt_m1, in1=rt_m2), reads=rr, writes=rr)
              GS3 = rt_gs.rearrange("p (t g) -> p t g", g=4)
              P.op('dve', lambda e: e.reduce_max(out=rt_gm, in_=GS3, axis=AX.X), reads=rr, writes=rr)
              P.op('dve', lambda e: e.tensor_tensor(out=rt_goh.rearrange("p (t g) -> p t g", g=4), in0=GS3,
                                                    in1=rt_gm.unsqueeze(2).to_broadcast([128, 16, 4]), op=ALU.is_equal), reads=rr, writes=rr)
              P.op('dve', lambda e: e.tensor_tensor(out=SL3, in0=B3, in1=rt_m2.unsqueeze(2).to_broadcast([128, 64, 4]), op=ALU.is_ge),
                   reads=rr, writes=rr)
              P.op('dve', lambda e: e.tensor_mul(out=SL3, in0=SL3, in1=rt_goh.unsqueeze(2).to_broadcast([128, 64, 4])), reads=rr, writes=rr)
              P.op('dve', lambda e: e.tensor_mul(out=rt_sel, in0=rt_sel, in1=sc2), reads=rr + [r_scores], writes=rr)
              P.op('dve', lambda e: e.reduce_sum(out=rt_den, in_=rt_sel.rearrange("p (t e) -> p t e", t=16), axis=AX.X), reads=rr, writes=rr)
              P.op('dve', lambda e: e.reciprocal(out=rt_den, in_=rt_den), reads=rr, writes=rr)
              P.op('dve', lambda e: e.tensor_mul(out=gates, in0=rt_sel.rearrange("p (t e) -> p t e", t=16),
                                                 in1=rt_den.unsqueeze(2).to_broadcast([128, 16, 16])), reads=rr, writes=[r_gates])

              if dbg == 'm5':
                  raise StopBuild()

              P.barrier()
              RC.reset(); RD.reset()
              wgu = [RD.bf(8 * 1024).rearrange("p (k c) -> p k c", k=8) for _ in range(2)]
              wd = [RD.bf(4 * 1024).rearrange("p (k c) -> p k c", k=4) for _ in range(2)]
              r_wgu = [Res(), Res()]
              r_wd = [Res(), Res()]
              hbuf = [RC.bf(4 * 512).rearrange("p (k c) -> p k c", k=4) for _ in range(2)]
              r_h = [Res(), Res()]
              sg = [RC.f32(512) for _ in range(2)]
              r_sg = [Res(), Res()]
              fi = [0]
              yi = [0]
              NEXP = 16 if dbg != 'e1' else 1
              def e_load(ex):
                  ws_ = ex % 2
                  wg_v = w_gate[l, ex].rearrange("(k p) f -> p k f", p=128)
                  wu_v = w_up[l, ex].rearrange("(k p) f -> p k f", p=128)
                  wd_v = w_down[l, ex].rearrange("(k p) f -> p k f", p=128)
                  for k in range(8):
                      stage_cast(wg_v[:, k, :], wgu[ws_][:, k, 0:512], [r_wgu[ws_]])
                      stage_cast(wu_v[:, k, :], wgu[ws_][:, k, 512:1024], [r_wgu[ws_]])
                  for k in range(4):
                      for h in range(2):
                          stage_cast(wd_v[:, k, h * 512:(h + 1) * 512], wd[ws_][:, k, h * 512:(h + 1) * 512], [r_wd[ws_]])

              def e_gu(j, fcs):
                  ex, tt = j // 4, j % 4
                  ws_ = ex % 2
                  cols = slice(tt * 512, (tt + 1) * 512)
                  hs = j % 2
                  H = hbuf[hs]
                  for fc in fcs:
                      f_ = fi[0] % 2
                      fi[0] += 1
                      gb, ub = 2 * f_, 2 * f_ + 1
                      for k in range(8):
                          P.op('pe', lambda e, k=k, fc=fc, gb=gb, cols=cols, ws_=ws_: e.matmul(
                              banks[gb], lhsT=wgu[ws_][:, k, fc * 128:(fc + 1) * 128], rhs=xT[:, k, cols],
                              start=(k == 0), stop=(k == 7)), reads=[r_wgu[ws_]] + r_xT[tt * 4:(tt + 1) * 4], writes=[rbank[gb]])
                      for k in range(8):
                          P.op('pe', lambda e, k=k, fc=fc, ub=ub, cols=cols, ws_=ws_: e.matmul(
                              banks[ub], lhsT=wgu[ws_][:, k, 512 + fc * 128:512 + (fc + 1) * 128], rhs=xT[:, k, cols],
                              start=(k == 0), stop=(k == 7)), reads=[r_wgu[ws_]] + r_xT[tt * 4:(tt + 1) * 4], writes=[rbank[ub]])
                      P.op('act', lambda e, f_=f_, gb=gb: e.activation(out=sg[f_], in_=banks[gb], func=AF.Silu),
                           reads=[rbank[gb]], writes=[r_sg[f_]])
                      P.op('dve', lambda e, f_=f_, ub=ub, fc=fc, H=H: e.tensor_mul(out=H[:, fc, :], in0=sg[f_], in1=banks[ub]),
                           reads=[r_sg[f_], rbank[ub]], writes=[r_hf[hs][fc]])

              def e_down(j):
                  ex, tt = j // 4, j % 4
                  ws_ = ex % 2
                  hs = j % 2
                  H = hbuf[hs]
                  for ts in range(4):
                      tb = tt * 4 + ts
                      for h in range(2):
                          yb = 4 + yi[0] % 2
                          yi[0] += 1
                          for fc in range(4):
                              P.op('pe', lambda e, fc=fc, ts=ts, h=h, yb=yb, H=H, ws_=ws_: e.matmul(
                                  banks[yb], lhsT=H[:, fc, ts * 128:(ts + 1) * 128], rhs=wd[ws_][:, fc, h * 512:(h + 1) * 512],
                                  start=(fc == 0), stop=(fc == 3)), reads=[r_hf[hs][fc], r_wd[ws_]], writes=[rbank[yb]])
                          P.op('dve', lambda e, tb=tb, h=h, yb=yb, ex=ex: e.scalar_tensor_tensor(
                              out=x1[:, tb, h * 512:(h + 1) * 512], in0=banks[yb], scalar=gates[:, tb, ex:ex + 1],
                              in1=x1[:, tb, h * 512:(h + 1) * 512], op0=ALU.mult, op1=ALU.add),
                              reads=[rbank[yb], r_gates, r_x1[tb]], writes=[r_x1[tb]])

              r_hf = [[Res() for _ in range(4)] for _ in range(2)]
              NJ = NEXP * 4
              e_load(0)
              e_gu(0, range(4))
              for j in range(NJ):
                  if j % 4 == 0 and j // 4 + 1 < NEXP:
                      e_load(j // 4 + 1)
                  if j + 1 < NJ:
                      e_gu(j + 1, [0, 1])
                  e_down(j)
                  if j + 1 < NJ:
                      e_gu(j + 1, [2, 3])

              P.barrier()
              RD.reset()
              g2b = RD.f32(1024)
              b2b = RD.f32(1024)
              r_g2 = Res()
              ybf = [RD.bf(1024) for _ in range(2)]
              r_ybf = [Res(), Res()]
              dma_in(g2b, ln2_g[l].partition_broadcast(128), 'd:gp', [r_g2])
              dma_in(b2b, ln2_b[l].partition_broadcast(128), 'd:gp', [r_g2])
              r_scr = Res()
              def ln2_fin(tb):
                  sl = tb % 2
                  ln_apply(x1[:, tb, :], r_x1[tb], g2b, b2b, r_g2, sl)
                  if last:
                      ro = Res()
                      r_outs.append(ro)
                      P.dma('sp', lambda e, tb=tb, s=s: e.dma_start(out=out[s, tb * 128:(tb + 1) * 128, :], in_=x1[:, tb, :]),
                            'd:out%d' % sl, reads=[r_x1[tb]], writes=[ro])
                  else:
                      P.dma('sp', lambda e, tb=tb: e.dma_start(out=scr[tb * 128:(tb + 1) * 128, :], in_=x1[:, tb, :]),
                            'd:out%d' % sl, reads=[r_x1[tb]], writes=[r_scr])
                      to_xT_bf16(x1[:, tb, :], r_x1[tb], ybf[sl], r_ybf[sl], tb, 6 + sl)

              ln_stats(x1[:, 0, :], r_x1[0], 0)
              for tb in range(NTB):
                  if tb + 1 < NTB:
                      ln_stats(x1[:, tb + 1, :], r_x1[tb + 1], (tb + 1) % 2)
                  ln2_fin(tb)

    except StopBuild:
        pass

    if dbg is not None:
        P.barrier()
        if dbg == 'm4':
            for k in range(4, 8):
                tmpf = f32v(X_o, 2048)
                rt_ = Res()
                P.op('pool', lambda e, k=k: e.tensor_copy(out=tmpf, in_=catT[:, k, :]), writes=[rt_])
                rd_ = dump(tmpf, [rt_])
                P.barrier()
        elif dbg == 'm3':
            for k in range(0, 8):
                tmpf = f32v(D_o, 2048)
                rt_ = Res()
                P.op('pool', lambda e, k=k: e.tensor_copy(out=tmpf, in_=catT[:, k, :]), writes=[rt_])
                dump(tmpf, [rt_])
                P.barrier()
        elif dbg == 'm3a':
            for qi_ in range(2):
                tmpf = f32v(D_o, 2048)
                rt_ = Res()
                P.op('pool', lambda e, qi_=qi_: e.tensor_copy(out=tmpf, in_=(kT_ if qi_ == 1 else qz[:, 0, :])), writes=[rt_])
                dump(tmpf, [rt_])
                P.barrier()
        elif dbg == 'm3b':
            dump(acc[:, 0, :], [])
            dump(acc[:, 1, :], [])
            P.barrier()
        elif dbg in ('m5', 'e1'):
            dump(x1[:, 0, :], [])
            dump(x1[:, 15, :], [])
            dump(gates.rearrange("p t e -> p (t e)"), [])
            dump(scores.rearrange("p t e -> p (t e)"), [])
            P.barrier()
        P.barrier()
    P.barrier()
    P.emit(nc, st)
    st.close()
    return nc


_CONSTS = None


def kernel(**inputs):
    global _CONSTS
    if _CONSTS is None:
        _CONSTS = _consts()
    n = 8
    nc = build(2, 2)
    x = np.ascontiguousarray(inputs["x"], dtype=np.float32)
    shared = {}
    for k, v in inputs.items():
        if k == "x":
            continue
        a = np.ascontiguousarray(np.asarray(v, dtype=np.float32))
        if k in ("gmlp_ln_g", "gmlp_ln_b"):
            a = a.reshape(2, 256)
        shared[k] = a
    shared.update(_CONSTS)
    in_maps = []
    for c in range(n):
        m = dict(shared)
        m["x"] = x[2 * c:2 * c + 2]
        in_maps.append(m)
    res = run_bass_kernel_spmd(nc, in_maps, core_ids=list(range(n)))
    return np.concatenate([r["out"] for r in res.results], axis=0).astype(np.float32)
```

```python
import os
import numpy as np
import ml_dtypes
from contextlib import ExitStack
import concourse.bass as bass
import concourse.mybir as mybir
from concourse.bass_utils import run_bass_kernel_spmd

F32 = mybir.dt.float32
BF16 = mybir.dt.bfloat16
AF = mybir.ActivationFunctionType
ALU = mybir.AluOpType
AX = mybir.AxisListType

S = 2048
D = 1024
NTB = 16
ALPHA = float(4 ** 0.25)
EPS = 1e-5
ENGS = ['pe', 'act', 'dve', 'pool', 'sp']


class StopBuild(Exception):
    pass


class Res:
    __slots__ = ('name', 'w', 'r')

    def __init__(self, name=''):
        self.name = name
        self.w = None
        self.r = {}


class Plan:
    def __init__(self):
        self.q = {e: [] for e in ENGS}
        self.cnt = {e: 0 for e in ENGS}
        self.known = {e: {} for e in ENGS}
        self.dma_keys = []

    def _collect(self, eng, reads, writes, is_dma):
        evs = {}

        def add(k, v, kind):
            if v is None:
                v = self.cnt[k]
            if k == eng and not is_dma:
                if eng == 'pe':
                    return
            if evs.get(k, 0) < v:
                evs[k] = v
        for r in reads:
            if r.w is not None:
                add(r.w[0], r.w[1], 'raw')
        for w in writes:
            if w.w is not None:
                add(w.w[0], w.w[1], 'waw')
            for k, v in w.r.items():
                add(k, v, 'war')
        waits = []
        kn = self.known[eng]
        for k, v in evs.items():
            if kn.get(k, 0) < v:
                waits.append((k, v))
                kn[k] = v
        return waits

    def op(self, eng, fn, reads=(), writes=()):
        waits = self._collect(eng, reads, writes, False)
        self.cnt[eng] += 1
        v = self.cnt[eng]
        self.q[eng].append((waits, fn, (eng, 1)))
        for r in reads:
            if r.r.get(eng, 0) < v:
                r.r[eng] = v
        for w in writes:
            w.w = (eng, v)
            w.r = {}

    def dma(self, eng, fn, key, reads=(), writes=()):
        if key not in self.cnt:
            self.cnt[key] = 0
            self.dma_keys.append(key)
        waits = self._collect(eng, reads, writes, True)
        self.cnt[key] += 16
        self.q[eng].append((waits, fn, (key, 16)))
        for r in reads:
            r.r[key] = None
        for w in writes:
            w.w = (key, None)
            w.r = {}

    def barrier(self):
        snap = dict(self.cnt)
        for e in ENGS:
            waits = []
            kn = self.known[e]
            for k, v in snap.items():
                if v == 0 or (k == e and e in ('pe', 'sp')):
                    continue
                if kn.get(k, 0) < v:
                    waits.append((k, v))
                    kn[k] = v
            self.q[e].append((waits, None, None))

    def emit(self, nc, stack):
        sems = {}
        for k in ENGS + self.dma_keys:
            sems[k] = stack.enter_context(nc.semaphore("s_" + k.replace(':', '_')))
        block = stack.enter_context(nc.Block())
        q = self.q

        def run(e, lst):
            for waits, fn, inc in lst:
                for k, v in waits:
                    e.wait_ge(sems[k], v)
                if fn is not None:
                    fn(e).then_inc(sems[inc[0]], inc[1])

        @block.tensor
        def _(e):
            run(e, q['pe'])

        @block.scalar
        def _(e):
            run(e, q['act'])

        @block.vector
        def _(e):
            run(e, q['dve'])

        @block.gpsimd
        def _(e):
            run(e, q['pool'])

        @block.sync
        def _(e):
            run(e, q['sp'])


def _consts():
    bf = ml_dtypes.bfloat16
    ki = np.arange(128)[:, None]
    qi = np.arange(128)[None, :]
    mA = (ki >= qi).astype(np.float32)
    mB = (ki <= qi).astype(np.float32)
    m3 = (np.abs(ki - qi) <= 64).astype(np.float32)
    masks = np.concatenate([mA, mB, mA, mB,
                            mB[:, 64:], mB[:, 64:],
                            mA[:, :64], mA[:, :64],
                            m3, m3], axis=1)
    masks = masks.astype(bf)
    identb = np.eye(128, dtype=np.float32).astype(bf)
    ident32 = np.eye(128, dtype=np.float32)
    perm = np.zeros((128, 128), np.float32)
    for base in (0, 64):
        for d in range(8):
            perm[base + d + 8, base + d] = -1.0
            perm[base + d - 0, base + d + 8] = 1.0
    perm = perm.astype(bf)
    pos = np.arange(S, dtype=np.float32)
    inv = np.power(np.float32(500000.0), -np.arange(8, dtype=np.float32) / 8).astype(np.float32)
    ang = pos[None, :] * inv[:, None]
    C = np.ones((128, S), np.float32)
    Sn = np.zeros((128, S), np.float32)
    for base in (0, 64):
        C[base:base + 8] = np.cos(ang)
        C[base + 8:base + 16] = np.cos(ang)
        Sn[base:base + 8] = np.sin(ang)
        Sn[base + 8:base + 16] = np.sin(ang)
    onesh = np.zeros((128, 2, 128), np.float32)
    onesh[:, 0, 0:64] = 1.0
    onesh[:, 1, 64:128] = 1.0
    rce = np.ones((128, 2, 16), np.float32)
    wins = {(0, 0): 2, (1, 0): 4, (0, 1): 8, (1, 1): 16}
    for (e, t), w in wins.items():
        left = w // 2
        right = w - 1 - left
        for i in range(8):
            tt = i
            cnt = min(tt + right + 1, S) - max(tt - left, 0)
            rce[e * 64:(e + 1) * 64, t, i] = 1.0 / cnt
            tt = S - 8 + i
            cnt = min(tt + right + 1, S) - max(tt - left, 0)
            rce[e * 64:(e + 1) * 64, t, 8 + i] = 1.0 / cnt
    return dict(c_masks=masks, c_identb=identb, c_ident32=ident32, c_perm=perm,
                c_ropeC=C.astype(bf), c_ropeS=Sn.astype(bf), c_onesh=onesh.astype(bf),
                c_rce=rce)


def build(NSEQ=2, DEPTH=2, dbg=None):
    nc = bass.Bass("TRN2", target_bir_lowering=False)
    dtn = nc.dram_tensor

    def din(name, shape, dt=F32):
        return dtn(name, list(shape), dt, kind="ExternalInput").ap()
    x = din("x", [NSEQ, S, D])
    w_in = din("w_in", [2, 1024, 2304])
    w_out = din("w_out", [2, 1024, 1024])
    gmlp_ln_g = din("gmlp_ln_g", [2, 256])
    gmlp_ln_b = din("gmlp_ln_b", [2, 256])
    gmlp_w_s = din("gmlp_w_s", [2, 4, 128, 128])
    gmlp_b_s = din("gmlp_b_s", [2, 4, 128])
    pool_w = din("pool_w", [2, 4, 64, 64])
    pool_scale = din("pool_scale", [2, 256])
    ln1_g = din("ln1_g", [2, 1024])
    ln1_b = din("ln1_b", [2, 1024])
    router_w = din("router_w", [1024, 16])
    router_bias = din("router_bias", [16])
    w_gate = din("w_gate", [2, 16, 1024, 512])
    w_up = din("w_up", [2, 16, 1024, 512])
    w_down = din("w_down", [2, 16, 512, 1024])
    ln2_g = din("ln2_g", [2, 1024])
    ln2_b = din("ln2_b", [2, 1024])
    c_masks = din("c_masks", [128, 1024], BF16)
    c_identb = din("c_identb", [128, 128], BF16)
    c_ident32 = din("c_ident32", [128, 128], F32)
    c_perm = din("c_perm", [128, 128], BF16)
    c_ropeC = din("c_ropeC", [128, S], BF16)
    c_ropeS = din("c_ropeS", [128, S], BF16)
    c_onesh = din("c_onesh", [128, 2, 128], BF16)
    c_rce = din("c_rce", [128, 2, 16], F32)
    out = dtn("out", [NSEQ, S, D], F32, kind="ExternalOutput").ap()
    scr = dtn("scr", [S, D], F32, kind="Internal").ap()
    dbg_out = None
    if dbg is not None:
        dbg_out = dtn("dbg", [128, 16384], F32, kind="ExternalOutput").ap()

    P = Plan()
    st = ExitStack()
    TOT = 204 * 1024
    arena = st.enter_context(nc.sbuf_tensor("arena", [128, TOT // 4], F32))
    banks = [st.enter_context(nc.psum_tensor("bank%d" % i, [128, 512], F32))[:] for i in range(8)]
    rbank = [Res("bank%d" % i) for i in range(8)]

    def f32v(off, n):
        assert off % 4 == 0
        return arena[:, off // 4: off // 4 + n]

    def bfv(off, n):
        assert off % 4 == 0 and n % 2 == 0
        return arena[:, off // 4: off // 4 + n // 2].bitcast(BF16)

    class Carve:
        def __init__(self, base, size):
            self.base, self.size, self.off = base, size, 0

        def reset(self):
            self.off = 0

        def f32(self, n):
            v = f32v(self.base + self.off, n)
            self.off += 4 * n
            assert self.off <= self.size, (self.off, self.size)
            return v

        def bf(self, n):
            n2 = (n + 1) // 2 * 2
            v = bfv(self.base + self.off, n2)
            self.off += 2 * n2
            assert self.off <= self.size, (self.off, self.size)
            return v[:, 0:n] if n2 != n else v

    o = 0
    XT_o = o; o += 32768
    X_o = o; o += 65536
    C_o = o; o += 32768
    D_o = o; o += 49152
    STG_o = o; o += 8192
    WT_o = o; o += 8192
    M_o = o
    misc = Carve(M_o, TOT - M_o)
    RX = Carve(X_o, 65536)
    RC = Carve(C_o, 32768)
    RD = Carve(D_o, 49152)

    xT = bfv(XT_o, 8 * S).rearrange("p (k t) -> p k t", k=8)
    r_xT = [[Res() for _ in range(NTB)] for _ in range(1)][0]
    stg = [f32v(STG_o + 2048 * i, 512) for i in range(4)]
    r_stg = [Res() for _ in range(4)]
    stg_i = [0]
    wt = [bfv(WT_o + 2048 * i, 1024).rearrange("p (k c) -> p k c", k=8) for i in range(4)]
    r_wt = [Res() for _ in range(4)]
    wt_i = [0]

    masks = misc.bf(1024)
    identb = misc.bf(128)
    ident32 = misc.f32(128)
    perm = misc.bf(128)
    onesh = misc.bf(256).rearrange("p (h c) -> p h c", h=2)
    rw32 = misc.f32(128).rearrange("p (k e) -> p k e", k=8)
    rbt = misc.f32(16)
    scores = misc.f32(256).rearrange("p (t e) -> p t e", t=16)
    gates = misc.f32(256).rearrange("p (t e) -> p t e", t=16)
    rt_b = misc.f32(256)
    rt_e = misc.f32(256)
    rt_b2 = misc.f32(256)
    rt_sel = misc.f32(256)
    rt_m1 = misc.f32(64)
    rt_m2 = misc.f32(64)
    rt_gs = misc.f32(64)
    rt_gm = misc.f32(16)
    rt_goh = misc.f32(64)
    rt_den = misc.f32(16)
    lnst = [misc.f32(12) for _ in range(2)]
    lnmv = [misc.f32(2) for _ in range(2)]
    lnrs = [misc.f32(1) for _ in range(2)]
    r_const = Res()
    r_scores = Res()
    r_gates = Res()
    r_rt = Res()
    r_ln = [Res(), Res()]

    def dma_in(dst, src, key, writes, reads=()):
        P.dma('sp', lambda e: e.dma_start(out=dst, in_=src), key, reads=reads, writes=writes)

    dma_in(masks, c_masks, 'd:c', [r_const])
    dma_in(identb, c_identb, 'd:c', [r_const])
    dma_in(ident32, c_ident32, 'd:c', [r_const])
    dma_in(perm, c_perm, 'd:c', [r_const])
    dma_in(onesh, c_onesh, 'd:c', [r_const])
    dma_in(rw32, router_w.rearrange("(k p) e -> p k e", p=128), 'd:c', [r_const])
    dma_in(rbt, router_bias.partition_broadcast(128), 'd:c', [r_const])

    def stage_cast(src, dst, r_dst_list, shape3=None, eng='act'):
        i = stg_i[0] % 4
        stg_i[0] += 1
        n = 1
        for d_ in dst.shape[1:]:
            n *= d_
        sv = stg[i][:, 0:n]
        if shape3 is not None:
            sv = sv.rearrange("p (a b) -> p a b", a=shape3)
        rs = r_stg[i]
        P.dma('sp', lambda e: e.dma_start(out=sv, in_=src), 'd:stg%d' % i, writes=[rs])
        if eng == 'act':
            P.op('act', lambda e: e.copy(out=dst, in_=sv), reads=[rs], writes=r_dst_list)
        else:
            P.op('pool', lambda e: e.tensor_copy(out=dst, in_=sv), reads=[rs], writes=r_dst_list)

    def load_ftile(l, c0):
        i = wt_i[0] % 4
        wt_i[0] += 1
        wv = w_in[l].rearrange("(k p) c -> p k c", p=128)
        for h in range(2):
            stage_cast(wv[:, 4 * h:4 * h + 4, c0:c0 + 128], wt[i][:, 4 * h:4 * h + 4, :], [r_wt[i]], shape3=4)
        return wt[i], r_wt[i]

    def tok_cols(tb):
        return slice(tb * 128, (tb + 1) * 128)

    def ln_stats(xt_ap, r_x, slot):
        stt, mv, rs = lnst[slot], lnmv[slot], lnrs[slot]
        rl = r_ln[slot]
        P.op('dve', lambda e: e.bn_stats(out=stt[:, 0:6], in_=xt_ap[:, 0:512]), reads=[r_x], writes=[rl])
        P.op('dve', lambda e: e.bn_stats(out=stt[:, 6:12], in_=xt_ap[:, 512:1024]), reads=[r_x], writes=[rl])
        P.op('dve', lambda e: e.bn_aggr(out=mv, in_=stt), reads=[rl], writes=[rl])
        P.op('act', lambda e: e.activation(out=rs, in_=mv[:, 1:2], func=AF.Ln, bias=EPS), reads=[rl], writes=[rl])
        P.op('act', lambda e: e.activation(out=rs, in_=rs, func=AF.Exp, scale=-0.5), reads=[rl], writes=[rl])

    def ln_apply(xt_ap, r_x, gtile, btile, r_gb, slot):
        mv, rs = lnmv[slot], lnrs[slot]
        rl = r_ln[slot]
        P.op('dve', lambda e: e.tensor_scalar(out=xt_ap, in0=xt_ap, scalar1=mv[:, 0:1], scalar2=rs[:, 0:1],
                                              op0=ALU.subtract, op1=ALU.mult), reads=[r_x, rl], writes=[r_x])
        P.op('dve', lambda e: e.tensor_mul(out=xt_ap, in0=xt_ap, in1=gtile), reads=[r_x, r_gb], writes=[r_x])
        P.op('dve', lambda e: e.tensor_add(out=xt_ap, in0=xt_ap, in1=btile), reads=[r_x, r_gb], writes=[r_x])

    def to_xT_bf16(src_f32, r_src, ybf, r_ybf, tb, bank):
        P.op('act', lambda e: e.copy(out=ybf, in_=src_f32), reads=[r_src], writes=[r_ybf])
        pb = banks[bank][:, 0:512].bitcast(BF16)
        for k in range(8):
            P.op('pe', lambda e, k=k: e.transpose(out=pb[:, k * 128:(k + 1) * 128], in_=ybf[:, k * 128:(k + 1) * 128],
                                                  identity=identb), reads=[r_ybf, r_const], writes=[rbank[bank]])
        P.op('act', lambda e: e.copy(out=xT[:, :, tok_cols(tb)], in_=pb.rearrange("p (k c) -> p k c", k=8)),
             reads=[rbank[bank]], writes=[r_xT[tb]])

    dump_i = [0]

    def dump(ap_f32_2d, reads):
        n = ap_f32_2d.shape[1]
        o0 = dump_i[0]
        dump_i[0] += n
        rr = Res()
        P.dma('sp', lambda e: e.dma_start(out=dbg_out[:, o0:o0 + n], in_=ap_f32_2d), 'd:dbg', reads=reads, writes=[rr])
        return rr

    r_outs = []

    try:
      for s in range(NSEQ):
          P.barrier()
          RD.reset()
          xin = [RD.f32(1024) for _ in range(2)]
          r_xin = [Res(), Res()]
          ybf = [RD.bf(1024) for _ in range(2)]
          r_ybf = [Res(), Res()]
          for tb in range(NTB):
              sl = tb % 2
              dma_in(xin[sl], x[s, tb * 128:(tb + 1) * 128, :], 'd:xin%d' % sl, [r_xin[sl]])
              to_xT_bf16(xin[sl], r_xin[sl], ybf[sl], r_ybf[sl], tb, 6 + sl)

          for l in range(DEPTH):
              x_src = x[s] if l == 0 else scr
              last = (l == DEPTH - 1)
              P.barrier()
              RX.reset(); RC.reset(); RD.reset()
              catT = RC.bf(8 * S).rearrange("p (k t) -> p k t", k=8)
              r_cat = [[Res() for _ in range(4)] for _ in range(8)]
              uT = RX.bf(2 * S).rearrange("p (k t) -> p k t", k=2)
              r_uT = [[Res() for _ in range(4)] for _ in range(2)]
              wvg = RX.bf(8 * 256).rearrange("p (k c) -> p k c", k=8)
              r_wvg = Res()
              ws32 = RX.f32(512).rearrange("p (g c) -> p g c", g=4)
              wsn = RX.bf(512).rearrange("p (g c) -> p g c", g=4)
              wsT = RX.bf(512).rearrange("p (g c) -> p g c", g=4)
              r_ws = Res()
              bsb = RX.f32(256).rearrange("p (t c) -> p t c", t=2)
              lng = RX.f32(256)
              lnb = RX.f32(256)
              r_gp = Res()
              wpb32 = RX.f32(256).rearrange("p (t c) -> p t c", t=2)
              wpbd = RX.bf(256).rearrange("p (t c) -> p t c", t=2)
              r_wpb = Res()
              psc = RX.f32(2)
              rce = RX.f32(32).rearrange("p (t c) -> p t c", t=2)
              gv = [RX.f32(512) for _ in range(2)]
              g2 = [RX.f32(512) for _ in range(2)]
              xc = [RX.f32(512) for _ in range(2)]
              vnp = [RX.bf(1024).rearrange("p (j t e c) -> p j t e c", j=2, t=2, e=2) for _ in range(2)]
              r_g = [Res(), Res()]
              r_g2 = [Res(), Res()]
              r_vnp = [Res(), Res()]
              sm = [[RX.f32(8) for _ in range(6)] for _ in range(2)]
              gtmp = [RX.f32(512) for _ in range(2)]
              r_gtmp = [Res(), Res()]
              etmp = RX.f32(8)
              pzp = RD.f32(2 * 2080).rearrange("p (t c) -> p t c", t=2)
              r_pzp = [Res(), Res()]
              sA = RD.f32(2080)
              sB = RD.f32(2080)
              r_sA, r_sB = Res(), Res()
              pooled = RD.bf(2 * S).rearrange("p (t c) -> p t c", t=2)
              r_pooled = [Res(), Res()]

              pre_ft = {}
              for c0_ in (1536, 1664, 2048, 2176):
                  pre_ft[c0_] = load_ftile(l, c0_)
              wv_ = w_in[l].rearrange("(k p) c -> p k c", p=128)
              for k2 in range(4):
                  stage_cast(wv_[:, 2 * k2:2 * k2 + 2, 1792:2048], wvg[:, 2 * k2:2 * k2 + 2, :], [r_wvg], shape3=2)
              for g in range(4):
                  dma_in(ws32[:, g, :], gmlp_w_s[l, g], 'd:gp', [r_ws])
              for t in range(2):
                  for e_ in range(2):
                      dma_in(bsb[e_ * 64:(e_ + 1) * 64, t, :], gmlp_b_s[l, 2 * t + e_].partition_broadcast(64), 'd:gp', [r_gp])
              dma_in(lng, gmlp_ln_g[l].partition_broadcast(128), 'd:gp', [r_gp])
              dma_in(lnb, gmlp_ln_b[l].partition_broadcast(128), 'd:gp', [r_gp])
              P.op('pool', lambda e: e.memset(wpb32, 0.0), writes=[r_wpb])
              for t in range(2):
                  for e_ in range(2):
                      dma_in(wpb32[e_ * 64:(e_ + 1) * 64, t, e_ * 64:(e_ + 1) * 64], pool_w[l, 2 * t + e_], 'd:gp', [r_wpb])
                  dma_in(psc[:, t:t + 1], pool_scale[l, t * 128:(t + 1) * 128].rearrange("(p o) -> p o", o=1), 'd:gp', [r_gp])
              dma_in(rce, c_rce, 'd:gp', [r_gp])
              P.op('pool', lambda e: e.tensor_copy(out=wpbd, in_=wpb32), reads=[r_wpb], writes=[r_wpb])
              P.op('pool', lambda e: e.tensor_copy(out=wsn, in_=ws32), reads=[r_ws], writes=[r_ws])
              wsp = banks[7][:, 0:256].bitcast(BF16)
              for g in range(4):
                  P.op('pe', lambda e, g=g: e.transpose(out=wsp[:, g * 128:(g + 1) * 128], in_=wsn[:, g, :], identity=identb),
                       reads=[r_ws, r_const], writes=[rbank[7]])
              P.op('act', lambda e: e.copy(out=wsT, in_=wsp.rearrange("p (g c) -> p g c", g=4)), reads=[rbank[7]], writes=[r_ws])
              for vi in range(2):
                  P.op('pool', lambda e, vi=vi: e.memset(vnp[vi], 0.0), writes=[r_vnp[vi]])
              P.op('pool', lambda e: e.memset(pzp, 0.0), writes=r_pzp)

              pbi = [0]

              def proj_ftile(c0, epilogue):
                  wtile, rw_ = pre_ft[c0]
                  for tt in range(4):
                      b = pbi[0] % 2
                      pbi[0] += 1
                      cols = slice(tt * 512, (tt + 1) * 512)
                      for k in range(8):
                          P.op('pe', lambda e, k=k, b=b, cols=cols: e.matmul(banks[b], lhsT=wtile[:, k, :], rhs=xT[:, k, cols],
                                                                             start=(k == 0), stop=(k == 7)),
                               reads=[rw_] + r_xT[tt * 4:(tt + 1) * 4], writes=[rbank[b]])
                      epilogue(tt, b, cols)

              for t in range(2):
                  def ep_u(tt, b, cols, t=t):
                      P.op('act', lambda e: e.activation(out=uT[:, t, cols], in_=banks[b], func=AF.Gelu_apprx_tanh),
                           reads=[rbank[b]], writes=[r_uT[t][tt]])
                  proj_ftile(1536 + t * 128, ep_u)
              pz_eps = {}
              for t in range(2):
                  def ep_p(tt, b, cols, t=t):
                      P.op('act', lambda e: e.copy(out=pzp[:, t, 16 + tt * 512:16 + (tt + 1) * 512], in_=banks[b]),
                           reads=[rbank[b]], writes=[r_pzp[t]])
                  pz_eps[t] = ep_p

              def pool_part(t):
                  Z = pzp[:, t, :]
                  P.op('dve', lambda e, Z=Z: e.tensor_add(out=sA[:, 1:2080], in0=Z[:, 1:2080], in1=Z[:, 0:2079]),
                       reads=[r_pzp[t]], writes=[r_sA])
                  P.op('dve', lambda e: e.tensor_add(out=sB[:, 2:2079], in0=sA[:, 3:2080], in1=sA[:, 1:2078]),
                       reads=[r_sA], writes=[r_sB])
                  if t == 1:
                      P.op('dve', lambda e: e.tensor_add(out=sA[:, 4:2077], in0=sB[:, 2:2075], in1=sB[:, 6:2079]),
                           reads=[r_sB], writes=[r_sA])
                      P.op('dve', lambda e: e.tensor_add(out=sB[:, 8:2073], in0=sA[:, 4:2069], in1=sA[:, 12:2077]),
                           reads=[r_sA], writes=[r_sB])
                  for e_ in range(2):
                      w_ = {(0, 0): 2, (1, 0): 4, (0, 1): 8, (1, 1): 16}[(e_, t)]
                      sb_ = sA if e_ == 0 else sB
                      rows = slice(e_ * 64, (e_ + 1) * 64)
                      P.op('dve', lambda e, sb_=sb_, rows=rows, w_=w_, Z=Z, t=t: e.scalar_tensor_tensor(
                          out=pooled[rows, t, :], in0=sb_[rows, 16:16 + S], scalar=1.0 / w_, in1=Z[rows, 16:16 + S],
                          op0=ALU.mult, op1=ALU.subtract), reads=[r_sA, r_sB, r_pzp[t]], writes=[r_pooled[t]])
                      for (c0, r0) in ((0, 0), (S - 8, 8)):
                          P.op('pool', lambda e, sb_=sb_, rows=rows, c0=c0, r0=r0, t=t: e.tensor_mul(
                              out=etmp[rows, :], in0=sb_[rows, 16 + c0:16 + c0 + 8], in1=rce[rows, t, r0:r0 + 8]),
                              reads=[r_sA, r_sB, r_gp], writes=[r_gtmp[0]])
                          P.op('pool', lambda e, rows=rows, c0=c0, Z=Z, t=t: e.tensor_sub(
                              out=pooled[rows, t, c0:c0 + 8], in0=etmp[rows, :], in1=Z[rows, 16 + c0:16 + c0 + 8]),
                              reads=[r_gtmp[0], r_pzp[t]], writes=[r_pooled[t]])
                  for tt in range(4):
                      b = pbi[0] % 2
                      pbi[0] += 1
                      cols = slice(tt * 512, (tt + 1) * 512)
                      P.op('pe', lambda e, b=b, cols=cols, t=t: e.matmul(banks[b], lhsT=wpbd[:, t, :], rhs=pooled[:, t, cols],
                                                                         start=True, stop=True),
                           reads=[r_wpb, r_pooled[t]], writes=[rbank[b]])
                      P.op('act', lambda e, b=b, cols=cols, t=t: e.activation(out=catT[:, 6 + t, cols], in_=banks[b], func=AF.Copy,
                                                                              scale=psc[:, t:t + 1]),
                           reads=[rbank[b], r_gp], writes=[r_cat[6 + t][tt]])

              def gm_stage_a(p):
                  vi = p % 2
                  pb = 2 + vi
                  for jj in range(2):
                      tb = 2 * p + jj
                      for k in range(8):
                          P.op('pe', lambda e, k=k, pb=pb, tb=tb, jj=jj: e.matmul(banks[pb][:, jj * 256:(jj + 1) * 256], lhsT=xT[:, k, tok_cols(tb)],
                                                                                 rhs=wvg[:, k, :], start=(k == 0), stop=(k == 7)),
                               reads=[r_xT[tb], r_wvg], writes=[rbank[pb]])
                  G, G2, XC = gv[vi], g2[vi], xc[vi]
                  s1, s2, mean, msq, var, rstd = sm[vi]
                  rg = r_g[vi]
                  rs_ = r_g2[vi]
                  P.op('act', lambda e, G=G, pb=pb: e.activation(out=G, in_=banks[pb], func=AF.Gelu_apprx_tanh),
                       reads=[rbank[pb]], writes=[rg])
                  P.op('act', lambda e, G=G, G2=G2: e.activation(out=G2, in_=G, func=AF.Square), reads=[rg], writes=[rs_])
                  G3 = G.rearrange("p (g c) -> p g c", g=8)
                  G23 = G2.rearrange("p (g c) -> p g c", g=8)
                  P.op('dve', lambda e, s1=s1, G3=G3: e.reduce_sum(out=s1, in_=G3, axis=AX.X), reads=[rg], writes=[rs_])
                  P.op('dve', lambda e, s2=s2, G23=G23: e.reduce_sum(out=s2, in_=G23, axis=AX.X), reads=[rs_], writes=[rs_])
                  P.op('dve', lambda e, mean=mean, s1=s1: e.tensor_scalar_mul(out=mean, in0=s1, scalar1=1.0 / 64), reads=[rs_], writes=[rs_])
                  P.op('dve', lambda e, msq=msq, mean=mean: e.tensor_mul(out=msq, in0=mean, in1=mean), reads=[rs_], writes=[rs_])
                  P.op('dve', lambda e, var=var, s2=s2, msq=msq: e.scalar_tensor_tensor(out=var, in0=s2, scalar=1.0 / 64, in1=msq,
                                                                                       op0=ALU.mult, op1=ALU.subtract), reads=[rs_], writes=[rs_])
                  P.op('act', lambda e, rstd=rstd, var=var: e.activation(out=rstd, in_=var, func=AF.Sqrt, bias=EPS), reads=[rs_], writes=[rs_])
                  P.op('dve', lambda e, rstd=rstd: e.reciprocal(out=rstd, in_=rstd), reads=[rs_], writes=[rs_])

              def gm_stage_b(p):
                  vi = p % 2
                  G, G2, XC = gv[vi], g2[vi], xc[vi]
                  s1, s2, mean, msq, var, rstd = sm[vi]
                  rg = r_g[vi]
                  rs_ = r_g2[vi]
                  G3 = G.rearrange("p (g c) -> p g c", g=8)
                  XC3 = XC.rearrange("p (g c) -> p g c", g=8)
                  P.op('dve', lambda e, XC3=XC3, G3=G3, mean=mean: e.tensor_sub(
                      out=XC3, in0=G3, in1=mean.unsqueeze(2).to_broadcast([128, 8, 64])), reads=[rg, rs_], writes=[rg])
                  P.op('dve', lambda e, XC3=XC3, rstd=rstd: e.tensor_mul(
                      out=XC3, in0=XC3, in1=rstd.unsqueeze(2).to_broadcast([128, 8, 64])), reads=[rg, rs_], writes=[rg])
                  XCj = XC.rearrange("p (j c) -> p j c", j=2)
                  P.op('pool', lambda e, XCj=XCj: e.tensor_mul(out=XCj, in0=XCj, in1=lng.unsqueeze(1).to_broadcast([128, 2, 256])),
                       reads=[rg, r_gp], writes=[rg])
                  XC5 = XC.rearrange("p (j t e c) -> p j t e c", j=2, t=2, e=2)
                  LB4 = lnb.rearrange("p (t e c) -> p t e c", t=2, e=2)
                  for e_ in range(2):
                      P.op('pool', lambda e, e_=e_, vi=vi, XC5=XC5: e.tensor_add(
                          out=vnp[vi][:, :, :, e_, e_ * 64:(e_ + 1) * 64], in0=XC5[:, :, :, e_, :],
                          in1=LB4[:, :, e_, :].unsqueeze(1).to_broadcast([128, 2, 2, 64])),
                          reads=[rg, r_gp], writes=[r_vnp[vi]])
                  grp = p // 2
                  for jj in range(2):
                      j = (p % 2) * 2 + jj
                      for t in range(2):
                          sb_ = 4 + t
                          for e_ in range(2):
                              P.op('pe', lambda e, t=t, e_=e_, vi=vi, sb_=sb_, j=j, jj=jj: e.matmul(
                                  banks[sb_][:, j * 128:(j + 1) * 128], lhsT=vnp[vi][:, jj, t, e_, :], rhs=wsT[:, 2 * t + e_, :],
                                  start=(e_ == 0), stop=(e_ == 1)), reads=[r_vnp[vi], r_ws], writes=[rbank[sb_]])
                  if p % 2 == 1:
                      cols = slice(grp * 512, (grp + 1) * 512)
                      for t in range(2):
                          sb_ = 4 + t
                          P.op('dve', lambda e, t=t, sb_=sb_: e.tensor_add(
                              out=gtmp[t].rearrange("p (j c) -> p j c", j=4), in0=banks[sb_].rearrange("p (j c) -> p j c", j=4),
                              in1=bsb[:, t, :].unsqueeze(1).to_broadcast([128, 4, 128])), reads=[rbank[sb_], r_gp], writes=[r_gtmp[t]])
                          P.op('pool', lambda e, t=t, cols=cols: e.tensor_mul(out=catT[:, 4 + t, cols], in0=gtmp[t], in1=uT[:, t, cols]),
                               reads=[r_gtmp[t], r_uT[t][grp]], writes=[r_cat[4 + t][grp]])

              gm_stage_a(0)
              extra = {0: lambda: proj_ftile(2048, pz_eps[0]), 1: lambda: proj_ftile(2176, pz_eps[1]),
                       2: lambda: pool_part(0), 4: lambda: pool_part(1)}
              for p in range(8):
                  if p in extra:
                      extra[p]()
                  if p + 1 < 8:
                      gm_stage_a(p + 1)
                  gm_stage_b(p)

              if dbg == 'm4':
                  raise StopBuild()

              P.barrier()
              RX.reset(); RD.reset()
              qz = RX.bf(2 * S).rearrange("p (h t) -> p h t", h=2)
              kT_ = RX.bf(S)
              r_qk = [[Res() for _ in range(4)] for _ in range(2)]
              vpad = [RX.bf(16 * 2 * 128).rearrange("p (b h c) -> p b h c", b=16, h=2) for _ in range(2)]
              r_vpad = [Res(), Res()]
              acc = RX.f32(2 * S).rearrange("p (a t) -> p a t", a=2)
              r_acc = Res()
              PT = [RX.bf(512) for _ in range(4)]
              r_PT = [Res() for _ in range(4)]
              qraw = [RD.bf(512) for _ in range(2)]
              r_qraw = [Res(), Res()]
              t1 = [RD.f32(512) for _ in range(2)]
              t2 = [RD.f32(512) for _ in range(2)]
              r_t = [Res(), Res()]
              ropeC = RD.bf(S)
              ropeS = RD.bf(S)
              r_rope = Res()
              dma_in(ropeC, c_ropeC, 'd:gp', [r_rope])
              dma_in(ropeS, c_ropeS, 'd:gp', [r_rope])
              for vi in range(2):
                  P.op('pool', lambda e, vi=vi: e.memset(vpad[vi], 0.0), writes=[r_vpad[vi]])
              P.op('pool', lambda e: e.memset(qz, 0.0), writes=r_qk[0])
              vslot = [0]
              ui = [0]
              nli = [0]
              for hp in range(4):
                  for qi_ in range(2):
                      wtile, rw_ = load_ftile(l, qi_ * 512 + hp * 128)
                      for tt in range(4):
                          sl = (qi_ * 4 + tt) % 2
                          cols = slice(tt * 512, (tt + 1) * 512)
                          pq = (qi_ * 4 + tt) % 4
                          pp = 6 + sl
                          for k in range(8):
                              P.op('pe', lambda e, k=k, cols=cols, wtile=wtile, pq=pq: e.matmul(banks[pq], lhsT=wtile[:, k, :], rhs=xT[:, k, cols],
                                                                                               start=(k == 0), stop=(k == 7)),
                                   reads=[rw_] + r_xT[tt * 4:(tt + 1) * 4], writes=[rbank[pq]])
                          P.op('act', lambda e, sl=sl, pq=pq: e.copy(out=qraw[sl], in_=banks[pq]), reads=[rbank[pq]], writes=[r_qraw[sl]])
                          P.op('dve', lambda e, sl=sl, cols=cols: e.tensor_mul(out=t1[sl], in0=qraw[sl], in1=ropeC[:, cols]),
                               reads=[r_qraw[sl], r_rope], writes=[r_t[sl]])
                          P.op('pe', lambda e, sl=sl, pp=pp: e.matmul(banks[pp], lhsT=perm, rhs=qraw[sl], start=True, stop=True),
                               reads=[r_qraw[sl], r_const], writes=[rbank[pp]])
                          P.op('dve', lambda e, sl=sl, cols=cols, pp=pp: e.tensor_mul(out=t2[sl], in0=banks[pp], in1=ropeS[:, cols]),
                               reads=[rbank[pp], r_rope], writes=[r_t[sl]])
                          if qi_ == 1:
                              P.op('dve', lambda e, sl=sl, cols=cols: e.tensor_add(out=kT_[:, cols], in0=t1[sl], in1=t2[sl]),
                                   reads=[r_t[sl]], writes=[r_qk[1][tt]])
                          else:
                              for hh in range(2):
                                  rows = slice(hh * 64, (hh + 1) * 64)
                                  P.op('dve', lambda e, sl=sl, cols=cols, hh=hh, rows=rows: e.tensor_add(
                                      out=qz[rows, hh, cols], in0=t1[sl][rows, :], in1=t2[sl][rows, :]),
                                       reads=[r_t[sl]], writes=[r_qk[0][tt]])
                  if dbg == 'm3a':
                      raise StopBuild()
                  wv, r_wv = load_ftile(l, 1024 + hp * 128)
                  P.op('pool', lambda e: e.memset(acc, 0.0), writes=[r_acc])
                  def do_cfg_v(dil, L):
                      vs = vslot[0] % 2
                      vslot[0] += 1
                      VP = vpad[vs]
                      nbr = L // 128
                      for g4 in range(4):
                          for j in range(4):
                              blk = g4 * 4 + j
                              r_, jb = blk // nbr, blk % nbr
                              t0 = r_ + dil * jb * 128
                              tsl = slice(t0, t0 + dil * 127 + 1, dil)
                              rd = r_xT if dil > 1 else [r_xT[blk]]
                              for k in range(8):
                                  P.op('pe', lambda e, k=k, tsl=tsl, j=j, wv=wv, vb=6 + g4 % 2: e.matmul(banks[vb][:, j * 128:(j + 1) * 128], lhsT=xT[:, k, tsl],
                                                                                 rhs=wv[:, k, :], start=(k == 0), stop=(k == 7)),
                                       reads=[r_wv] + rd, writes=[rbank[6 + g4 % 2]])
                          b6 = banks[6 + g4 % 2].rearrange("p (j h c) -> p j h c", j=4, h=2)
                          for h in range(2):
                              P.op('act', lambda e, g4=g4, h=h, VP=VP, b6=b6: e.copy(
                                  out=VP[:, g4 * 4:(g4 + 1) * 4, h, h * 64:(h + 1) * 64], in_=b6[:, :, h, :]),
                                  reads=[rbank[6 + g4 % 2]], writes=[r_vpad[vs]])
                      return VP, vs, nbr

                  def do_cfg_units(dil, L, VP, vs, nbr):
                      allunits = []
                      for r_ in range(dil):
                          if L == 128:
                              units = [(0, 128, [(0, 768)])]
                          else:
                              units = [(0, 64, [(0, 512)])]
                              for i in range(L // 128 - 1):
                                  units.append((128 * i + 64, 128, [(i, 0), (i + 1, 128)]))
                              units.append((L - 64, 64, [(L // 128 - 1, 640)]))
                          for (qa, nq, kbs) in units:
                              allunits.append((r_, qa, nq, kbs))

                      def rcols(r_, a, n, dil=dil):
                          t0 = r_ + dil * a
                          return slice(t0, t0 + dil * (n - 1) + 1, dil)

                      def stage_s(un):
                          r_, qa, nq, kbs = un
                          u_ = ui[0]
                          ui[0] += 1
                          sbk = u_ % 4
                          nk = len(kbs)
                          tot = 2 * nk * nq
                          m0 = kbs[0][1]
                          mbase = 0 if nk == 2 else m0
                          qc_ = rcols(r_, qa, nq)
                          for hh in range(2):
                              for ki_, (kb, _) in enumerate(kbs):
                                  off = (hh * nk + ki_) * nq
                                  kc_ = rcols(r_, kb * 128, 128)
                                  P.op('pe', lambda e, hh=hh, kc_=kc_, qc_=qc_, off=off, nq=nq, sbk=sbk: e.matmul(
                                      banks[sbk][:, off:off + nq], lhsT=kT_[:, kc_], rhs=qz[:, hh, qc_],
                                      start=True, stop=True), reads=r_qk[0] + r_qk[1], writes=[rbank[sbk]])
                          P.op('act', lambda e, sbk=sbk, tot=tot: e.activation(out=PT[sbk][:, 0:tot], in_=banks[sbk][:, 0:tot],
                                                                              func=AF.Exp, scale=0.125),
                               reads=[rbank[sbk]], writes=[r_PT[sbk]])
                          P.op('pool' if u_ % 2 == 0 else 'dve', lambda e, sbk=sbk, tot=tot, mbase=mbase: e.tensor_mul(
                              out=PT[sbk][:, 0:tot], in0=PT[sbk][:, 0:tot], in1=masks[:, mbase:mbase + tot]),
                               reads=[r_PT[sbk], r_const], writes=[r_PT[sbk]])
                          return sbk

                      pvst = {'col': 0, 'first': 0, 'nbk': 4}

                      def stage_pv(un, sbk, VP=VP, vs=vs, nbr=nbr, L=L):
                          r_, qa, nq, kbs = un
                          nk = len(kbs)
                          if pvst['col'] == 0:
                              nli[0] += 1
                              pvst['nbk'] = 4 + nli[0] % 2
                              pvst['first'] = qa
                          nbk = pvst['nbk']
                          col = pvst['col']
                          n_mm = 2 * nk
                          i_mm = 0
                          for hh in range(2):
                              for ki_, (kb, _) in enumerate(kbs):
                                  off = (hh * nk + ki_) * nq
                                  blk = r_ * nbr + kb
                                  P.op('pe', lambda e, hh=hh, blk=blk, off=off, nq=nq, col=col, i_mm=i_mm, n_mm=n_mm, nbk=nbk, sbk=sbk: e.matmul(
                                      banks[nbk][:, col:col + nq], lhsT=VP[:, blk, hh, :], rhs=PT[sbk][:, off:off + nq],
                                      start=(i_mm == 0), stop=(i_mm == n_mm - 1)), reads=[r_PT[sbk], r_vpad[vs]], writes=[rbank[nbk]])
                                  i_mm += 1
                          i_mm = 0
                          for hh in range(2):
                              for ki_, (kb, _) in enumerate(kbs):
                                  off = (hh * nk + ki_) * nq
                                  P.op('pe', lambda e, hh=hh, off=off, nq=nq, col=col, i_mm=i_mm, n_mm=n_mm, nbk=nbk, sbk=sbk: e.matmul(
                                      banks[nbk][:, 256 + col:256 + col + nq], lhsT=onesh[:, hh, :], rhs=PT[sbk][:, off:off + nq],
                                      start=(i_mm == 0), stop=(i_mm == n_mm - 1)), reads=[r_PT[sbk], r_const], writes=[rbank[nbk]])
                                  i_mm += 1
                          col += nq
                          is_last = (qa + nq == L)
                          if col + 128 > 256 or is_last:
                              acols = rcols(r_, pvst['first'], col)
                              P.op('dve', lambda e, acols=acols, col=col, nbk=nbk: e.tensor_add(
                                  out=acc[:, :, acols], in0=acc[:, :, acols],
                                  in1=banks[nbk].rearrange("p (a c) -> p a c", a=2)[:, :, 0:col]),
                                   reads=[rbank[nbk], r_acc], writes=[r_acc])
                              col = 0
                          pvst['col'] = col

                      LA = 3
                      sb_of = {}
                      for i in range(len(allunits) + LA):
                          if i < len(allunits):
                              sb_of[i] = stage_s(allunits[i])
                          if i - LA >= 0:
                              stage_pv(allunits[i - LA], sb_of[i - LA])
                      if dbg == 'm3b':
                          raise StopBuild()

                  cfgs = ((1, 2048), (4, 512), (16, 128))
                  vinfo = do_cfg_v(*cfgs[0])
                  for ci in range(3):
                      nxt = do_cfg_v(*cfgs[ci + 1]) if ci + 1 < 3 else None
                      do_cfg_units(cfgs[ci][0], cfgs[ci][1], *vinfo)
                      vinfo = nxt
                  P.op('act', lambda e: e.activation(out=acc[:, 1, :], in_=acc[:, 1, :], func=AF.Ln), reads=[r_acc], writes=[r_acc])
                  P.op('act', lambda e: e.activation(out=acc[:, 1, :], in_=acc[:, 1, :], func=AF.Exp, scale=-1.0), reads=[r_acc], writes=[r_acc])
                  P.op('dve', lambda e, hp=hp: e.tensor_mul(out=catT[:, hp, :], in0=acc[:, 0, :], in1=acc[:, 1, :]), reads=[r_acc], writes=r_cat[hp])

              if dbg == 'm3':
                  raise StopBuild()

              P.barrier()
              RX.reset(); RD.reset()
              x1 = RX.f32(NTB * D).rearrange("p (t c) -> p t c", t=NTB)
              r_x1 = [Res() for _ in range(NTB)]
              wout = RD.bf(8 * 1024).rearrange("p (k c) -> p k c", k=8)
              r_wout = Res()
              xin = [RD.f32(1024) for _ in range(2)]
              r_xin = [Res(), Res()]
              xr32 = [RD.f32(1024).rearrange("p (k c) -> p k c", k=8) for _ in range(2)]
              r_xr = [Res(), Res()]
              g1b = RD.f32(1024)
              b1b = RD.f32(1024)
              r_g1 = Res()
              dma_in(g1b, ln1_g[l].partition_broadcast(128), 'd:gp', [r_g1])
              dma_in(b1b, ln1_b[l].partition_broadcast(128), 'd:gp', [r_g1])
              wo_ = w_out[l].rearrange("(k p) c -> p k c", p=128)
              for k in range(8):
                  for h in range(2):
                      stage_cast(wo_[:, k, h * 512:(h + 1) * 512], wout[:, k, h * 512:(h + 1) * 512], [r_wout])
              def m5_a(tb):
                  sl = tb % 2
                  dma_in(xin[sl], x_src[tb * 128:(tb + 1) * 128, :], 'd:xin%d' % sl, [r_xin[sl]],
                         reads=[r_scr] if l > 0 else ())
                  for h in range(2):
                      ob = 2 * sl + h
                      for k in range(8):
                          P.op('pe', lambda e, k=k, h=h, ob=ob, tb=tb: e.matmul(banks[ob], lhsT=catT[:, k, tok_cols(tb)],
                                                                                rhs=wout[:, k, h * 512:(h + 1) * 512],
                                                                                start=(k == 0), stop=(k == 7)),
                               reads=[r_cat[k][tb // 4], r_wout], writes=[rbank[ob]])
                      P.op('dve', lambda e, h=h, ob=ob, tb=tb, sl=sl: e.scalar_tensor_tensor(
                          out=x1[:, tb, h * 512:(h + 1) * 512], in0=xin[sl][:, h * 512:(h + 1) * 512], scalar=ALPHA,
                          in1=banks[ob], op0=ALU.mult, op1=ALU.add), reads=[r_xin[sl], rbank[ob]], writes=[r_x1[tb]])

              def m5_a2(tb):
                  ln_stats(x1[:, tb, :], r_x1[tb], tb % 2)

              def m5_b1(tb):
                  sl = tb % 2
                  ln_apply(x1[:, tb, :], r_x1[tb], g1b, b1b, r_g1, sl)
                  for h in range(2):
                      tbk = 4 + h
                      for k4 in range(4):
                          k = h * 4 + k4
                          P.op('pe', lambda e, k=k, k4=k4, tbk=tbk, tb=tb: e.transpose(
                              out=banks[tbk][:, k4 * 128:(k4 + 1) * 128], in_=x1[:, tb, k * 128:(k + 1) * 128], identity=ident32),
                              reads=[r_x1[tb], r_const], writes=[rbank[tbk]])
                      b3 = banks[tbk].rearrange("p (k c) -> p k c", k=4)
                      P.op('dve', lambda e, h=h, sl=sl, b3=b3: e.tensor_copy(out=xr32[sl][:, h * 4:(h + 1) * 4, :], in_=b3),
                           reads=[rbank[tbk]], writes=[r_xr[sl]])
                      P.op('act', lambda e, h=h, tb=tb, sl=sl: e.copy(out=xT[:, h * 4:(h + 1) * 4, tok_cols(tb)], in_=xr32[sl][:, h * 4:(h + 1) * 4, :]),
                           reads=[r_xr[sl]], writes=[r_xT[tb]])

              def m5_b2(tb):
                  sl = tb % 2
                  for k in range(8):
                      P.op('pe', lambda e, k=k, sl=sl: e.matmul(banks[6][:, 0:16], lhsT=xr32[sl][:, k, :], rhs=rw32[:, k, :],
                                                                start=(k == 0), stop=(k == 7)),
                           reads=[r_xr[sl], r_const], writes=[rbank[6]])
                  P.op('act', lambda e, tb=tb: e.activation(out=scores[:, tb, :], in_=banks[6][:, 0:16], func=AF.Exp, scale=-1.0),
                       reads=[rbank[6]], writes=[r_scores])
                  P.op('act', lambda e, tb=tb: e.mul(out=x1[:, tb, :], in_=x1[:, tb, :], mul=ALPHA),
                       reads=[r_x1[tb]], writes=[r_x1[tb]])

              for i in range(-3, NTB):
                  if 0 <= i + 3 < NTB:
                      m5_a(i + 3)
                  if 0 <= i + 2 < NTB:
                      m5_a2(i + 2)
                  if 0 <= i + 1 < NTB:
                      m5_b1(i + 1)
                  if 0 <= i:
                      m5_b2(i)

              sc2 = scores.rearrange("p t e -> p (t e)")
              P.op('dve', lambda e: e.tensor_scalar_add(out=sc2, in0=sc2, scalar1=1.0), reads=[r_scores], writes=[r_scores])
              P.op('dve', lambda e: e.reciprocal(out=sc2, in_=sc2), reads=[r_scores], writes=[r_scores])
              B3 = rt_b.rearrange("p (a c) -> p a c", c=4)
              E3 = rt_e.rearrange("p (a c) -> p a c", c=4)
              B23 = rt_b2.rearrange("p (a c) -> p a c", c=4)
              SL3 = rt_sel.rearrange("p (a c) -> p a c", c=4)
              rr = [r_rt]
              P.op('dve', lambda e: e.tensor_add(out=rt_b.rearrange("p (t e) -> p t e", t=16), in0=scores,
                                                 in1=rbt.unsqueeze(1).to_broadcast([128, 16, 16])), reads=[r_scores, r_const], writes=rr)
              P.op('dve', lambda e: e.reduce_max(out=rt_m1, in_=B3, axis=AX.X), reads=rr, writes=rr)
              P.op('dve', lambda e: e.tensor_tensor(out=E3, in0=B3, in1=rt_m1.unsqueeze(2).to_broadcast([128, 64, 4]), op=ALU.is_equal),
                   reads=rr, writes=rr)
              P.op('dve', lambda e: e.scalar_tensor_tensor(out=rt_b2, in0=rt_e, scalar=-1e9, in1=rt_b, op0=ALU.mult, op1=ALU.add),
                   reads=rr, writes=rr)
              P.op('dve', lambda e: e.reduce_max(out=rt_m2, in_=B23, axis=AX.X), reads=rr, writes=rr)
              P.op('dve', lambda e: e.tensor_add(out=rt_gs, in0=rt_m1, in1=rt_m2), reads=rr, writes=rr)
              GS3 = rt_gs.rearrange("p (t g) -> p t g", g=4)
              P.op('dve', lambda e: e.reduce_max(out=rt_gm, in_=GS3, axis=AX.X), reads=rr, writes=rr)
              P.op('dve', lambda e: e.tensor_tensor(out=rt_goh.rearrange("p (t g) -> p t g", g=4), in0=GS3,
                                                    in1=rt_gm.unsqueeze(2).to_broadcast([128, 16, 4]), op=ALU.is_equal), reads=rr, writes=rr)
              P.op('dve', lambda e: e.tensor_tensor(out=SL3, in0=B3, in1=rt_m2.unsqueeze(2).to_broadcast([128, 64, 4]), op=ALU.is_ge),
                   reads=rr, writes=rr)
              P.op('dve', lambda e: e.tensor_mul(out=SL3, in0=SL3, in1=rt_goh.unsqueeze(2).to_broadcast([128, 64, 4])), reads=rr, writes=rr)
              P.op('dve', lambda e: e.tensor_mul(out=rt_sel, in0=rt_sel, in1=sc2), reads=rr + [r_scores], writes=rr)
              P.op('dve', lambda e: e.reduce_sum(out=rt_den, in_=rt_sel.rearrange("p (t e) -> p t e", t=16), axis=AX.X), reads=rr, writes=rr)
              P.op('dve', lambda e: e.reciprocal(out=rt_den, in_=rt_den), reads=rr, writes=rr)
              P.op('dve', lambda e: e.tensor_mul(out=gates, in0=rt_sel.rearrange("p (t e) -> p t e", t=16),
                                                 in1=rt_den.unsqueeze(2).to_broadcast([128, 16, 16])), reads=rr, writes=[r_gates])

              if dbg == 'm5':
                  raise StopBuild()

              P.barrier()
              RC.reset(); RD.reset()
              wgu = [RD.bf(8 * 1024).rearrange("p (k c) -> p k c", k=8) for _ in range(2)]
              wd = [RD.bf(4 * 1024).rearrange("p (k c) -> p k c", k=4) for _ in range(2)]
              r_wgu = [Res(), Res()]
              r_wd = [Res(), Res()]
              hbuf = [RC.bf(4 * 512).rearrange("p (k c) -> p k c", k=4) for _ in range(2)]
              r_h = [Res(), Res()]
              sg = [RC.f32(512) for _ in range(2)]
              r_sg = [Res(), Res()]
              fi = [0]
              yi = [0]
              NEXP = 16 if dbg != 'e1' else 1
              def e_load(ex):
                  ws_ = ex % 2
                  ceng = 'act' if ex == 0 else 'pool'
                  wg_v = w_gate[l, ex].rearrange("(k p) f -> p k f", p=128)
                  wu_v = w_up[l, ex].rearrange("(k p) f -> p k f", p=128)
                  wd_v = w_down[l, ex].rearrange("(k p) f -> p k f", p=128)
                  for k in range(8):
                      stage_cast(wg_v[:, k, :], wgu[ws_][:, k, 0:512], [r_wgu[ws_]], eng=ceng)
                      stage_cast(wu_v[:, k, :], wgu[ws_][:, k, 512:1024], [r_wgu[ws_]], eng=ceng)
                  for k in range(4):
                      for h in range(2):
                          stage_cast(wd_v[:, k, h * 512:(h + 1) * 512], wd[ws_][:, k, h * 512:(h + 1) * 512], [r_wd[ws_]], eng=ceng)

              def e_gu(j, fcs):
                  ex, tt = j // 4, j % 4
                  ws_ = ex % 2
                  cols = slice(tt * 512, (tt + 1) * 512)
                  hs = j % 2
                  H = hbuf[hs]
                  for fc in fcs:
                      f_ = fi[0] % 2
                      fi[0] += 1
                      gb, ub = 2 * f_, 2 * f_ + 1
                      for k in range(8):
                          P.op('pe', lambda e, k=k, fc=fc, gb=gb, cols=cols, ws_=ws_: e.matmul(
                              banks[gb], lhsT=wgu[ws_][:, k, fc * 128:(fc + 1) * 128], rhs=xT[:, k, cols],
                              start=(k == 0), stop=(k == 7)), reads=[r_wgu[ws_]] + r_xT[tt * 4:(tt + 1) * 4], writes=[rbank[gb]])
                      for k in range(8):
                          P.op('pe', lambda e, k=k, fc=fc, ub=ub, cols=cols, ws_=ws_: e.matmul(
                              banks[ub], lhsT=wgu[ws_][:, k, 512 + fc * 128:512 + (fc + 1) * 128], rhs=xT[:, k, cols],
                              start=(k == 0), stop=(k == 7)), reads=[r_wgu[ws_]] + r_xT[tt * 4:(tt + 1) * 4], writes=[rbank[ub]])
                      P.op('act', lambda e, f_=f_, gb=gb: e.activation(out=sg[f_], in_=banks[gb], func=AF.Silu),
                           reads=[rbank[gb]], writes=[r_sg[f_]])
                      P.op('dve', lambda e, f_=f_, ub=ub, fc=fc, H=H: e.tensor_mul(out=H[:, fc, :], in0=sg[f_], in1=banks[ub]),
                           reads=[r_sg[f_], rbank[ub]], writes=[r_hf[hs][fc]])

              def e_down(j):
                  ex, tt = j // 4, j % 4
                  ws_ = ex % 2
                  hs = j % 2
                  H = hbuf[hs]
                  for ts in range(4):
                      tb = tt * 4 + ts
                      for h in range(2):
                          yb = 4 + yi[0] % 2
                          yi[0] += 1
                          for fc in range(4):
                              P.op('pe', lambda e, fc=fc, ts=ts, h=h, yb=yb, H=H, ws_=ws_: e.matmul(
                                  banks[yb], lhsT=H[:, fc, ts * 128:(ts + 1) * 128], rhs=wd[ws_][:, fc, h * 512:(h + 1) * 512],
                                  start=(fc == 0), stop=(fc == 3)), reads=[r_hf[hs][fc], r_wd[ws_]], writes=[rbank[yb]])
                          P.op('dve', lambda e, tb=tb, h=h, yb=yb, ex=ex: e.scalar_tensor_tensor(
                              out=x1[:, tb, h * 512:(h + 1) * 512], in0=banks[yb], scalar=gates[:, tb, ex:ex + 1],
                              in1=x1[:, tb, h * 512:(h + 1) * 512], op0=ALU.mult, op1=ALU.add),
                              reads=[rbank[yb], r_gates, r_x1[tb]], writes=[r_x1[tb]])

              r_hf = [[Res() for _ in range(4)] for _ in range(2)]
              NJ = NEXP * 4
              e_load(0)
              e_gu(0, range(4))
              for j in range(NJ):
                  if j % 4 == 0 and j // 4 + 1 < NEXP:
                      e_load(j // 4 + 1)
                  if j + 1 < NJ:
                      e_gu(j + 1, [0, 1])
                  e_down(j)
                  if j + 1 < NJ:
                      e_gu(j + 1, [2, 3])

              P.barrier()
              RD.reset()
              g2b = RD.f32(1024)
              b2b = RD.f32(1024)
              r_g2 = Res()
              ybf = [RD.bf(1024) for _ in range(2)]
              r_ybf = [Res(), Res()]
              dma_in(g2b, ln2_g[l].partition_broadcast(128), 'd:gp', [r_g2])
              dma_in(b2b, ln2_b[l].partition_broadcast(128), 'd:gp', [r_g2])
              r_scr = Res()
              def ln2_fin(tb):
                  sl = tb % 2
                  ln_apply(x1[:, tb, :], r_x1[tb], g2b, b2b, r_g2, sl)
                  if last:
                      ro = Res()
                      r_outs.append(ro)
                      P.dma('sp', lambda e, tb=tb, s=s: e.dma_start(out=out[s, tb * 128:(tb + 1) * 128, :], in_=x1[:, tb, :]),
                            'd:out%d' % sl, reads=[r_x1[tb]], writes=[ro])
                  else:
                      P.dma('sp', lambda e, tb=tb: e.dma_start(out=scr[tb * 128:(tb + 1) * 128, :], in_=x1[:, tb, :]),
                            'd:out%d' % sl, reads=[r_x1[tb]], writes=[r_scr])
                      to_xT_bf16(x1[:, tb, :], r_x1[tb], ybf[sl], r_ybf[sl], tb, 6 + sl)

              ln_stats(x1[:, 0, :], r_x1[0], 0)
              for tb in range(NTB):
                  if tb + 1 < NTB:
                      ln_stats(x1[:, tb + 1, :], r_x1[tb + 1], (tb + 1) % 2)
                  ln2_fin(tb)

    except StopBuild:
        pass

    if dbg is not None:
        P.barrier()
        if dbg == 'm4':
            for k in range(4, 8):
                tmpf = f32v(X_o, 2048)
                rt_ = Res()
                P.op('pool', lambda e, k=k: e.tensor_copy(out=tmpf, in_=catT[:, k, :]), writes=[rt_])
                rd_ = dump(tmpf, [rt_])
                P.barrier()
        elif dbg == 'm3':
            for k in range(0, 8):
                tmpf = f32v(D_o, 2048)
                rt_ = Res()
                P.op('pool', lambda e, k=k: e.tensor_copy(out=tmpf, in_=catT[:, k, :]), writes=[rt_])
                dump(tmpf, [rt_])
                P.barrier()
        elif dbg == 'm3a':
            for qi_ in range(2):
                tmpf = f32v(D_o, 2048)
                rt_ = Res()
                P.op('pool', lambda e, qi_=qi_: e.tensor_copy(out=tmpf, in_=(kT_ if qi_ == 1 else qz[:, 0, :])), writes=[rt_])
                dump(tmpf, [rt_])
                P.barrier()
        elif dbg == 'm3b':
            dump(acc[:, 0, :], [])
            dump(acc[:, 1, :], [])
            P.barrier()
        elif dbg in ('m5', 'e1'):
            dump(x1[:, 0, :], [])
            dump(x1[:, 15, :], [])
            dump(gates.rearrange("p t e -> p (t e)"), [])
            dump(scores.rearrange("p t e -> p (t e)"), [])
            P.barrier()
        P.barrier()
    P.barrier()
    P.emit(nc, st)
    st.close()
    return nc


_CONSTS = None


def kernel(**inputs):
    global _CONSTS
    if _CONSTS is None:
        _CONSTS = _consts()
    n = 8
    nc = build(2, 2)
    x = np.ascontiguousarray(inputs["x"], dtype=np.float32)
    shared = {}
    for k, v in inputs.items():
        if k == "x":
            continue
        a = np.ascontiguousarray(np.asarray(v, dtype=np.float32))
        if k in ("gmlp_ln_g", "gmlp_ln_b"):
            a = a.reshape(2, 256)
        shared[k] = a
    shared.update(_CONSTS)
    in_maps = []
    for c in range(n):
        m = dict(shared)
        m["x"] = x[2 * c:2 * c + 2]
        in_maps.append(m)
    res = run_bass_kernel_spmd(nc, in_maps, core_ids=list(range(n)))
    return np.concatenate([r["out"] for r in res.results], axis=0).astype(np.float32)
```

```python
import os
import numpy as np
import ml_dtypes
from contextlib import ExitStack
import concourse.bass as bass
import concourse.mybir as mybir
from concourse.bass_utils import run_bass_kernel_spmd

F32 = mybir.dt.float32
BF16 = mybir.dt.bfloat16
AF = mybir.ActivationFunctionType
ALU = mybir.AluOpType
AX = mybir.AxisListType

S = 2048
D = 1024
NTB = 16
ALPHA = float(4 ** 0.25)
EPS = 1e-5
ENGS = ['pe', 'act', 'dve', 'pool', 'sp']


class StopBuild(Exception):
    pass


class Res:
    __slots__ = ('name', 'w', 'r')

    def __init__(self, name=''):
        self.name = name
        self.w = None
        self.r = {}


class Plan:
    def __init__(self):
        self.q = {e: [] for e in ENGS}
        self.cnt = {e: 0 for e in ENGS}
        self.known = {e: {} for e in ENGS}
        self.dma_keys = []

    def _collect(self, eng, reads, writes, is_dma):
        evs = {}

        def add(k, v, kind):
            if v is None:
                v = self.cnt[k]
            if k == eng and not is_dma:
                if eng == 'pe':
                    return
            if evs.get(k, 0) < v:
                evs[k] = v
        for r in reads:
            if r.w is not None:
                add(r.w[0], r.w[1], 'raw')
        for w in writes:
            if w.w is not None:
                add(w.w[0], w.w[1], 'waw')
            for k, v in w.r.items():
                add(k, v, 'war')
        waits = []
        kn = self.known[eng]
        for k, v in evs.items():
            if kn.get(k, 0) < v:
                waits.append((k, v))
                kn[k] = v
        return waits

    def op(self, eng, fn, reads=(), writes=()):
        waits = self._collect(eng, reads, writes, False)
        self.cnt[eng] += 1
        v = self.cnt[eng]
        self.q[eng].append((waits, fn, (eng, 1)))
        for r in reads:
            if r.r.get(eng, 0) < v:
                r.r[eng] = v
        for w in writes:
            w.w = (eng, v)
            w.r = {}

    def dma(self, eng, fn, key, reads=(), writes=()):
        if key not in self.cnt:
            self.cnt[key] = 0
            self.dma_keys.append(key)
        waits = self._collect(eng, reads, writes, True)
        self.cnt[key] += 16
        self.q[eng].append((waits, fn, (key, 16)))
        for r in reads:
            r.r[key] = None
        for w in writes:
            w.w = (key, None)
            w.r = {}

    def barrier(self):
        snap = dict(self.cnt)
        for e in ENGS:
            waits = []
            kn = self.known[e]
            for k, v in snap.items():
                if v == 0 or (k == e and e in ('pe', 'sp')):
                    continue
                if kn.get(k, 0) < v:
                    waits.append((k, v))
                    kn[k] = v
            self.q[e].append((waits, None, None))

    def emit(self, nc, stack):
        sems = {}
        for k in ENGS + self.dma_keys:
            sems[k] = stack.enter_context(nc.semaphore("s_" + k.replace(':', '_')))
        block = stack.enter_context(nc.Block())
        q = self.q

        def run(e, lst):
            for waits, fn, inc in lst:
                for k, v in waits:
                    e.wait_ge(sems[k], v)
                if fn is not None:
                    fn(e).then_inc(sems[inc[0]], inc[1])

        @block.tensor
        def _(e):
            run(e, q['pe'])

        @block.scalar
        def _(e):
            run(e, q['act'])

        @block.vector
        def _(e):
            run(e, q['dve'])

        @block.gpsimd
        def _(e):
            run(e, q['pool'])

        @block.sync
        def _(e):
            run(e, q['sp'])


def _consts():
    bf = ml_dtypes.bfloat16
    ki = np.arange(128)[:, None]
    qi = np.arange(128)[None, :]
    mA = (ki >= qi).astype(np.float32)
    mB = (ki <= qi).astype(np.float32)
    m3 = (np.abs(ki - qi) <= 64).astype(np.float32)
    masks = np.concatenate([mA, mB, mA, mB,
                            mB[:, 64:], mB[:, 64:],
                            mA[:, :64], mA[:, :64],
                            m3, m3], axis=1)
    masks = masks.astype(bf)
    identb = np.eye(128, dtype=np.float32).astype(bf)
    ident32 = np.eye(128, dtype=np.float32)
    perm = np.zeros((128, 128), np.float32)
    for base in (0, 64):
        for d in range(8):
            perm[base + d + 8, base + d] = -1.0
            perm[base + d - 0, base + d + 8] = 1.0
    perm = perm.astype(bf)
    pos = np.arange(S, dtype=np.float32)
    inv = np.power(np.float32(500000.0), -np.arange(8, dtype=np.float32) / 8).astype(np.float32)
    ang = pos[None, :] * inv[:, None]
    C = np.ones((128, S), np.float32)
    Sn = np.zeros((128, S), np.float32)
    for base in (0, 64):
        C[base:base + 8] = np.cos(ang)
        C[base + 8:base + 16] = np.cos(ang)
        Sn[base:base + 8] = np.sin(ang)
        Sn[base + 8:base + 16] = np.sin(ang)
    onesh = np.zeros((128, 2, 128), np.float32)
    onesh[:, 0, 0:64] = 1.0
    onesh[:, 1, 64:128] = 1.0
    rce = np.ones((128, 2, 16), np.float32)
    wins = {(0, 0): 2, (1, 0): 4, (0, 1): 8, (1, 1): 16}
    for (e, t), w in wins.items():
        left = w // 2
        right = w - 1 - left
        for i in range(8):
            tt = i
            cnt = min(tt + right + 1, S) - max(tt - left, 0)
            rce[e * 64:(e + 1) * 64, t, i] = 1.0 / cnt
            tt = S - 8 + i
            cnt = min(tt + right + 1, S) - max(tt - left, 0)
            rce[e * 64:(e + 1) * 64, t, 8 + i] = 1.0 / cnt
    return dict(c_masks=masks, c_identb=identb, c_ident32=ident32, c_perm=perm,
                c_ropeC=C.astype(bf), c_ropeS=Sn.astype(bf), c_onesh=onesh.astype(bf),
                c_rce=rce)


def build(NSEQ=2, DEPTH=2, dbg=None):
    nc = bass.Bass("TRN2", target_bir_lowering=False)
    dtn = nc.dram_tensor

    def din(name, shape, dt=F32):
        return dtn(name, list(shape), dt, kind="ExternalInput").ap()
    x = din("x", [NSEQ, S, D])
    w_in = din("w_in", [2, 1024, 2304])
    w_out = din("w_out", [2, 1024, 1024])
    gmlp_ln_g = din("gmlp_ln_g", [2, 256])
    gmlp_ln_b = din("gmlp_ln_b", [2, 256])
    gmlp_w_s = din("gmlp_w_s", [2, 4, 128, 128])
    gmlp_b_s = din("gmlp_b_s", [2, 4, 128])
    pool_w = din("pool_w", [2, 4, 64, 64])
    pool_scale = din("pool_scale", [2, 256])
    ln1_g = din("ln1_g", [2, 1024])
    ln1_b = din("ln1_b", [2, 1024])
    router_w = din("router_w", [1024, 16])
    router_bias = din("router_bias", [16])
    w_gate = din("w_gate", [2, 16, 1024, 512])
    w_up = din("w_up", [2, 16, 1024, 512])
    w_down = din("w_down", [2, 16, 512, 1024])
    ln2_g = din("ln2_g", [2, 1024])
    ln2_b = din("ln2_b", [2, 1024])
    c_masks = din("c_masks", [128, 1024], BF16)
    c_identb = din("c_identb", [128, 128], BF16)
    c_ident32 = din("c_ident32", [128, 128], F32)
    c_perm = din("c_perm", [128, 128], BF16)
    c_ropeC = din("c_ropeC", [128, S], BF16)
    c_ropeS = din("c_ropeS", [128, S], BF16)
    c_onesh = din("c_onesh", [128, 2, 128], BF16)
    c_rce = din("c_rce", [128, 2, 16], F32)
    out = dtn("out", [NSEQ, S, D], F32, kind="ExternalOutput").ap()
    scr = dtn("scr", [S, D], F32, kind="Internal").ap()
    dbg_out = None
    if dbg is not None:
        dbg_out = dtn("dbg", [128, 16384], F32, kind="ExternalOutput").ap()

    P = Plan()
    st = ExitStack()
    TOT = 204 * 1024
    arena = st.enter_context(nc.sbuf_tensor("arena", [128, TOT // 4], F32))
    banks = [st.enter_context(nc.psum_tensor("bank%d" % i, [128, 512], F32))[:] for i in range(8)]
    rbank = [Res("bank%d" % i) for i in range(8)]

    def f32v(off, n):
        assert off % 4 == 0
        return arena[:, off // 4: off // 4 + n]

    def bfv(off, n):
        assert off % 4 == 0 and n % 2 == 0
        return arena[:, off // 4: off // 4 + n // 2].bitcast(BF16)

    class Carve:
        def __init__(self, base, size):
            self.base, self.size, self.off = base, size, 0

        def reset(self):
            self.off = 0

        def f32(self, n):
            v = f32v(self.base + self.off, n)
            self.off += 4 * n
            assert self.off <= self.size, (self.off, self.size)
            return v

        def bf(self, n):
            n2 = (n + 1) // 2 * 2
            v = bfv(self.base + self.off, n2)
            self.off += 2 * n2
            assert self.off <= self.size, (self.off, self.size)
            return v[:, 0:n] if n2 != n else v

    o = 0
    XT_o = o; o += 32768
    X_o = o; o += 65536
    C_o = o; o += 32768
    D_o = o; o += 49152
    STG_o = o; o += 8192
    WT_o = o; o += 8192
    M_o = o
    misc = Carve(M_o, TOT - M_o)
    RX = Carve(X_o, 65536)
    RC = Carve(C_o, 32768)
    RD = Carve(D_o, 49152)

    xT = bfv(XT_o, 8 * S).rearrange("p (k t) -> p k t", k=8)
    r_xT = [[Res() for _ in range(NTB)] for _ in range(1)][0]
    stg = [f32v(STG_o + 2048 * i, 512) for i in range(4)]
    r_stg = [Res() for _ in range(4)]
    stg_i = [0]
    wt = [bfv(WT_o + 2048 * i, 1024).rearrange("p (k c) -> p k c", k=8) for i in range(4)]
    r_wt = [Res() for _ in range(4)]
    wt_i = [0]

    masks = misc.bf(1024)
    identb = misc.bf(128)
    ident32 = misc.f32(128)
    perm = misc.bf(128)
    onesh = misc.bf(256).rearrange("p (h c) -> p h c", h=2)
    rw32 = misc.f32(128).rearrange("p (k e) -> p k e", k=8)
    rbt = misc.f32(16)
    scores = misc.f32(256).rearrange("p (t e) -> p t e", t=16)
    gates = misc.f32(256).rearrange("p (t e) -> p t e", t=16)
    rt_b = misc.f32(256)
    rt_e = misc.f32(256)
    rt_b2 = misc.f32(256)
    rt_sel = misc.f32(256)
    rt_m1 = misc.f32(64)
    rt_m2 = misc.f32(64)
    rt_gs = misc.f32(64)
    rt_gm = misc.f32(16)
    rt_goh = misc.f32(64)
    rt_den = misc.f32(16)
    lnst = [misc.f32(12) for _ in range(2)]
    lnmv = [misc.f32(2) for _ in range(2)]
    lnrs = [misc.f32(1) for _ in range(2)]
    r_const = Res()
    r_scores = Res()
    r_gates = Res()
    r_rt = Res()
    r_ln = [Res(), Res()]

    def dma_in(dst, src, key, writes, reads=()):
        P.dma('sp', lambda e: e.dma_start(out=dst, in_=src), key, reads=reads, writes=writes)

    dma_in(masks, c_masks, 'd:c', [r_const])
    dma_in(identb, c_identb, 'd:c', [r_const])
    dma_in(ident32, c_ident32, 'd:c', [r_const])
    dma_in(perm, c_perm, 'd:c', [r_const])
    dma_in(onesh, c_onesh, 'd:c', [r_const])
    dma_in(rw32, router_w.rearrange("(k p) e -> p k e", p=128), 'd:c', [r_const])
    dma_in(rbt, router_bias.partition_broadcast(128), 'd:c', [r_const])

    def stage_cast(src, dst, r_dst_list, shape3=None, eng='act'):
        i = stg_i[0] % 4
        stg_i[0] += 1
        n = 1
        for d_ in dst.shape[1:]:
            n *= d_
        sv = stg[i][:, 0:n]
        if shape3 is not None:
            sv = sv.rearrange("p (a b) -> p a b", a=shape3)
        rs = r_stg[i]
        P.dma('sp', lambda e: e.dma_start(out=sv, in_=src), 'd:stg%d' % i, writes=[rs])
        if eng == 'act':
            P.op('act', lambda e: e.copy(out=dst, in_=sv), reads=[rs], writes=r_dst_list)
        else:
            P.op('pool', lambda e: e.tensor_copy(out=dst, in_=sv), reads=[rs], writes=r_dst_list)

    def load_ftile(l, c0):
        i = wt_i[0] % 4
        wt_i[0] += 1
        wv = w_in[l].rearrange("(k p) c -> p k c", p=128)
        for h in range(2):
            stage_cast(wv[:, 4 * h:4 * h + 4, c0:c0 + 128], wt[i][:, 4 * h:4 * h + 4, :], [r_wt[i]], shape3=4)
        return wt[i], r_wt[i]

    def tok_cols(tb):
        return slice(tb * 128, (tb + 1) * 128)

    def ln_stats(xt_ap, r_x, slot):
        stt, mv, rs = lnst[slot], lnmv[slot], lnrs[slot]
        rl = r_ln[slot]
        P.op('dve', lambda e: e.bn_stats(out=stt[:, 0:6], in_=xt_ap[:, 0:512]), reads=[r_x], writes=[rl])
        P.op('dve', lambda e: e.bn_stats(out=stt[:, 6:12], in_=xt_ap[:, 512:1024]), reads=[r_x], writes=[rl])
        P.op('dve', lambda e: e.bn_aggr(out=mv, in_=stt), reads=[rl], writes=[rl])
        P.op('act', lambda e: e.activation(out=rs, in_=mv[:, 1:2], func=AF.Ln, bias=EPS), reads=[rl], writes=[rl])
        P.op('act', lambda e: e.activation(out=rs, in_=rs, func=AF.Exp, scale=-0.5), reads=[rl], writes=[rl])

    def ln_apply(xt_ap, r_x, gtile, btile, r_gb, slot):
        mv, rs = lnmv[slot], lnrs[slot]
        rl = r_ln[slot]
        P.op('dve', lambda e: e.tensor_scalar(out=xt_ap, in0=xt_ap, scalar1=mv[:, 0:1], scalar2=rs[:, 0:1],
                                              op0=ALU.subtract, op1=ALU.mult), reads=[r_x, rl], writes=[r_x])
        P.op('dve', lambda e: e.tensor_mul(out=xt_ap, in0=xt_ap, in1=gtile), reads=[r_x, r_gb], writes=[r_x])
        P.op('dve', lambda e: e.tensor_add(out=xt_ap, in0=xt_ap, in1=btile), reads=[r_x, r_gb], writes=[r_x])

    def to_xT_bf16(src_f32, r_src, ybf, r_ybf, tb, bank):
        P.op('act', lambda e: e.copy(out=ybf, in_=src_f32), reads=[r_src], writes=[r_ybf])
        pb = banks[bank][:, 0:512].bitcast(BF16)
        for k in range(8):
            P.op('pe', lambda e, k=k: e.transpose(out=pb[:, k * 128:(k + 1) * 128], in_=ybf[:, k * 128:(k + 1) * 128],
                                                  identity=identb), reads=[r_ybf, r_const], writes=[rbank[bank]])
        P.op('act', lambda e: e.copy(out=xT[:, :, tok_cols(tb)], in_=pb.rearrange("p (k c) -> p k c", k=8)),
             reads=[rbank[bank]], writes=[r_xT[tb]])

    dump_i = [0]

    def dump(ap_f32_2d, reads):
        n = ap_f32_2d.shape[1]
        o0 = dump_i[0]
        dump_i[0] += n
        rr = Res()
        P.dma('sp', lambda e: e.dma_start(out=dbg_out[:, o0:o0 + n], in_=ap_f32_2d), 'd:dbg', reads=reads, writes=[rr])
        return rr

    r_outs = []

    try:
      for s in range(NSEQ):
          P.barrier()
          RD.reset()
          xin = [RD.f32(1024) for _ in range(2)]
          r_xin = [Res(), Res()]
          ybf = [RD.bf(1024) for _ in range(2)]
          r_ybf = [Res(), Res()]
          for tb in range(NTB):
              sl = tb % 2
              dma_in(xin[sl], x[s, tb * 128:(tb + 1) * 128, :], 'd:xin%d' % sl, [r_xin[sl]])
              to_xT_bf16(xin[sl], r_xin[sl], ybf[sl], r_ybf[sl], tb, 6 + sl)

          for l in range(DEPTH):
              x_src = x[s] if l == 0 else scr
              last = (l == DEPTH - 1)
              P.barrier()
              RX.reset(); RC.reset(); RD.reset()
              catT = RC.bf(8 * S).rearrange("p (k t) -> p k t", k=8)
              r_cat = [[Res() for _ in range(4)] for _ in range(8)]
              uT = RX.bf(2 * S).rearrange("p (k t) -> p k t", k=2)
              r_uT = [[Res() for _ in range(4)] for _ in range(2)]
              wvg = RX.bf(8 * 256).rearrange("p (k c) -> p k c", k=8)
              r_wvg = Res()
              ws32 = RX.f32(512).rearrange("p (g c) -> p g c", g=4)
              wsn = RX.bf(512).rearrange("p (g c) -> p g c", g=4)
              wsT = RX.bf(512).rearrange("p (g c) -> p g c", g=4)
              r_ws = Res()
              bsb = RX.f32(256).rearrange("p (t c) -> p t c", t=2)
              lng = RX.f32(256)
              lnb = RX.f32(256)
              r_gp = Res()
              wpb32 = RX.f32(256).rearrange("p (t c) -> p t c", t=2)
              wpbd = RX.bf(256).rearrange("p (t c) -> p t c", t=2)
              r_wpb = Res()
              psc = RX.f32(2)
              rce = RX.f32(32).rearrange("p (t c) -> p t c", t=2)
              gv = [RX.f32(512) for _ in range(2)]
              g2 = [RX.f32(512) for _ in range(2)]
              xc = [RX.f32(512) for _ in range(2)]
              vnp = [RX.bf(1024).rearrange("p (j t e c) -> p j t e c", j=2, t=2, e=2) for _ in range(2)]
              r_g = [Res(), Res()]
              r_g2 = [Res(), Res()]
              r_vnp = [Res(), Res()]
              sm = [[RX.f32(8) for _ in range(6)] for _ in range(2)]
              gtmp = [RX.f32(512) for _ in range(2)]
              r_gtmp = [Res(), Res()]
              etmp = RX.f32(8)
              pzp = RD.f32(2 * 2080).rearrange("p (t c) -> p t c", t=2)
              r_pzp = [Res(), Res()]
              sA = RD.f32(2080)
              sB = RD.f32(2080)
              r_sA, r_sB = Res(), Res()
              pooled = RD.bf(2 * S).rearrange("p (t c) -> p t c", t=2)
              r_pooled = [Res(), Res()]

              pre_ft = {}
              for c0_ in (1536, 1664, 2048, 2176):
                  pre_ft[c0_] = load_ftile(l, c0_)
              wv_ = w_in[l].rearrange("(k p) c -> p k c", p=128)
              for k2 in range(4):
                  stage_cast(wv_[:, 2 * k2:2 * k2 + 2, 1792:2048], wvg[:, 2 * k2:2 * k2 + 2, :], [r_wvg], shape3=2)
              for g in range(4):
                  dma_in(ws32[:, g, :], gmlp_w_s[l, g], 'd:gp', [r_ws])
              for t in range(2):
                  for e_ in range(2):
                      dma_in(bsb[e_ * 64:(e_ + 1) * 64, t, :], gmlp_b_s[l, 2 * t + e_].partition_broadcast(64), 'd:gp', [r_gp])
              dma_in(lng, gmlp_ln_g[l].partition_broadcast(128), 'd:gp', [r_gp])
              dma_in(lnb, gmlp_ln_b[l].partition_broadcast(128), 'd:gp', [r_gp])
              P.op('pool', lambda e: e.memset(wpb32, 0.0), writes=[r_wpb])
              for t in range(2):
                  for e_ in range(2):
                      dma_in(wpb32[e_ * 64:(e_ + 1) * 64, t, e_ * 64:(e_ + 1) * 64], pool_w[l, 2 * t + e_], 'd:gp', [r_wpb])
                  dma_in(psc[:, t:t + 1], pool_scale[l, t * 128:(t + 1) * 128].rearrange("(p o) -> p o", o=1), 'd:gp', [r_gp])
              dma_in(rce, c_rce, 'd:gp', [r_gp])
              P.op('pool', lambda e: e.tensor_copy(out=wpbd, in_=wpb32), reads=[r_wpb], writes=[r_wpb])
              P.op('pool', lambda e: e.tensor_copy(out=wsn, in_=ws32), reads=[r_ws], writes=[r_ws])
              wsp = banks[7][:, 0:256].bitcast(BF16)
              for g in range(4):
                  P.op('pe', lambda e, g=g: e.transpose(out=wsp[:, g * 128:(g + 1) * 128], in_=wsn[:, g, :], identity=identb),
                       reads=[r_ws, r_const], writes=[rbank[7]])
              P.op('act', lambda e: e.copy(out=wsT, in_=wsp.rearrange("p (g c) -> p g c", g=4)), reads=[rbank[7]], writes=[r_ws])
              for vi in range(2):
                  P.op('pool', lambda e, vi=vi: e.memset(vnp[vi], 0.0), writes=[r_vnp[vi]])
              P.op('pool', lambda e: e.memset(pzp, 0.0), writes=r_pzp)

              pbi = [0]

              def proj_ftile(c0, epilogue):
                  wtile, rw_ = pre_ft[c0]
                  for tt in range(4):
                      b = pbi[0] % 2
                      pbi[0] += 1
                      cols = slice(tt * 512, (tt + 1) * 512)
                      for k in range(8):
                          P.op('pe', lambda e, k=k, b=b, cols=cols: e.matmul(banks[b], lhsT=wtile[:, k, :], rhs=xT[:, k, cols],
                                                                             start=(k == 0), stop=(k == 7)),
                               reads=[rw_] + r_xT[tt * 4:(tt + 1) * 4], writes=[rbank[b]])
                      epilogue(tt, b, cols)

              for t in range(2):
                  def ep_u(tt, b, cols, t=t):
                      P.op('act', lambda e: e.activation(out=uT[:, t, cols], in_=banks[b], func=AF.Gelu_apprx_tanh),
                           reads=[rbank[b]], writes=[r_uT[t][tt]])
                  proj_ftile(1536 + t * 128, ep_u)
              pz_eps = {}
              for t in range(2):
                  def ep_p(tt, b, cols, t=t):
                      P.op('act', lambda e: e.copy(out=pzp[:, t, 16 + tt * 512:16 + (tt + 1) * 512], in_=banks[b]),
                           reads=[rbank[b]], writes=[r_pzp[t]])
                  pz_eps[t] = ep_p

              def pool_part(t):
                  Z = pzp[:, t, :]
                  P.op('dve', lambda e, Z=Z: e.tensor_add(out=sA[:, 1:2080], in0=Z[:, 1:2080], in1=Z[:, 0:2079]),
                       reads=[r_pzp[t]], writes=[r_sA])
                  P.op('dve', lambda e: e.tensor_add(out=sB[:, 2:2079], in0=sA[:, 3:2080], in1=sA[:, 1:2078]),
                       reads=[r_sA], writes=[r_sB])
                  if t == 1:
                      P.op('dve', lambda e: e.tensor_add(out=sA[:, 4:2077], in0=sB[:, 2:2075], in1=sB[:, 6:2079]),
                           reads=[r_sB], writes=[r_sA])
                      P.op('dve', lambda e: e.tensor_add(out=sB[:, 8:2073], in0=sA[:, 4:2069], in1=sA[:, 12:2077]),
                           reads=[r_sA], writes=[r_sB])
                  for e_ in range(2):
                      w_ = {(0, 0): 2, (1, 0): 4, (0, 1): 8, (1, 1): 16}[(e_, t)]
                      sb_ = sA if e_ == 0 else sB
                      rows = slice(e_ * 64, (e_ + 1) * 64)
                      P.op('dve', lambda e, sb_=sb_, rows=rows, w_=w_, Z=Z, t=t: e.scalar_tensor_tensor(
                          out=pooled[rows, t, :], in0=sb_[rows, 16:16 + S], scalar=1.0 / w_, in1=Z[rows, 16:16 + S],
                          op0=ALU.mult, op1=ALU.subtract), reads=[r_sA, r_sB, r_pzp[t]], writes=[r_pooled[t]])
                      for (c0, r0) in ((0, 0), (S - 8, 8)):
                          P.op('pool', lambda e, sb_=sb_, rows=rows, c0=c0, r0=r0, t=t: e.tensor_mul(
                              out=etmp[rows, :], in0=sb_[rows, 16 + c0:16 + c0 + 8], in1=rce[rows, t, r0:r0 + 8]),
                              reads=[r_sA, r_sB, r_gp], writes=[r_gtmp[0]])
                          P.op('pool', lambda e, rows=rows, c0=c0, Z=Z, t=t: e.tensor_sub(
                              out=pooled[rows, t, c0:c0 + 8], in0=etmp[rows, :], in1=Z[rows, 16 + c0:16 + c0 + 8]),
                              reads=[r_gtmp[0], r_pzp[t]], writes=[r_pooled[t]])
                  for tt in range(4):
                      b = pbi[0] % 2
                      pbi[0] += 1
                      cols = slice(tt * 512, (tt + 1) * 512)
                      P.op('pe', lambda e, b=b, cols=cols, t=t: e.matmul(banks[b], lhsT=wpbd[:, t, :], rhs=pooled[:, t, cols],
                                                                         start=True, stop=True),
                           reads=[r_wpb, r_pooled[t]], writes=[rbank[b]])
                      P.op('act', lambda e, b=b, cols=cols, t=t: e.activation(out=catT[:, 6 + t, cols], in_=banks[b], func=AF.Copy,
                                                                              scale=psc[:, t:t + 1]),
                           reads=[rbank[b], r_gp], writes=[r_cat[6 + t][tt]])

              def gm_stage_a(p):
                  vi = p % 2
                  pb = 2 + vi
                  for jj in range(2):
                      tb = 2 * p + jj
                      for k in range(8):
                          P.op('pe', lambda e, k=k, pb=pb, tb=tb, jj=jj: e.matmul(banks[pb][:, jj * 256:(jj + 1) * 256], lhsT=xT[:, k, tok_cols(tb)],
                                                                                 rhs=wvg[:, k, :], start=(k == 0), stop=(k == 7)),
                               reads=[r_xT[tb], r_wvg], writes=[rbank[pb]])
                  G, G2, XC = gv[vi], g2[vi], xc[vi]
                  s1, s2, mean, msq, var, rstd = sm[vi]
                  rg = r_g[vi]
                  rs_ = r_g2[vi]
                  P.op('act', lambda e, G=G, pb=pb: e.activation(out=G, in_=banks[pb], func=AF.Gelu_apprx_tanh),
                       reads=[rbank[pb]], writes=[rg])
                  P.op('act', lambda e, G=G, G2=G2: e.activation(out=G2, in_=G, func=AF.Square), reads=[rg], writes=[rs_])
                  G3 = G.rearrange("p (g c) -> p g c", g=8)
                  G23 = G2.rearrange("p (g c) -> p g c", g=8)
                  P.op('dve', lambda e, s1=s1, G3=G3: e.reduce_sum(out=s1, in_=G3, axis=AX.X), reads=[rg], writes=[rs_])
                  P.op('dve', lambda e, s2=s2, G23=G23: e.reduce_sum(out=s2, in_=G23, axis=AX.X), reads=[rs_], writes=[rs_])
                  P.op('dve', lambda e, mean=mean, s1=s1: e.tensor_scalar_mul(out=mean, in0=s1, scalar1=1.0 / 64), reads=[rs_], writes=[rs_])
                  P.op('dve', lambda e, msq=msq, mean=mean: e.tensor_mul(out=msq, in0=mean, in1=mean), reads=[rs_], writes=[rs_])
                  P.op('dve', lambda e, var=var, s2=s2, msq=msq: e.scalar_tensor_tensor(out=var, in0=s2, scalar=1.0 / 64, in1=msq,
                                                                                       op0=ALU.mult, op1=ALU.subtract), reads=[rs_], writes=[rs_])
                  P.op('act', lambda e, rstd=rstd, var=var: e.activation(out=rstd, in_=var, func=AF.Sqrt, bias=EPS), reads=[rs_], writes=[rs_])
                  P.op('dve', lambda e, rstd=rstd: e.reciprocal(out=rstd, in_=rstd), reads=[rs_], writes=[rs_])

              def gm_stage_b(p):
                  vi = p % 2
                  G, G2, XC = gv[vi], g2[vi], xc[vi]
                  s1, s2, mean, msq, var, rstd = sm[vi]
                  rg = r_g[vi]
                  rs_ = r_g2[vi]
                  G3 = G.rearrange("p (g c) -> p g c", g=8)
                  XC3 = XC.rearrange("p (g c) -> p g c", g=8)
                  P.op('dve', lambda e, XC3=XC3, G3=G3, mean=mean: e.tensor_sub(
                      out=XC3, in0=G3, in1=mean.unsqueeze(2).to_broadcast([128, 8, 64])), reads=[rg, rs_], writes=[rg])
                  P.op('dve', lambda e, XC3=XC3, rstd=rstd: e.tensor_mul(
                      out=XC3, in0=XC3, in1=rstd.unsqueeze(2).to_broadcast([128, 8, 64])), reads=[rg, rs_], writes=[rg])
                  XCj = XC.rearrange("p (j c) -> p j c", j=2)
                  P.op('dve', lambda e, XCj=XCj: e.tensor_mul(out=XCj, in0=XCj, in1=lng.unsqueeze(1).to_broadcast([128, 2, 256])),
                       reads=[rg, r_gp], writes=[rg])
                  XC5 = XC.rearrange("p (j t e c) -> p j t e c", j=2, t=2, e=2)
                  LB4 = lnb.rearrange("p (t e c) -> p t e c", t=2, e=2)
                  for e_ in range(2):
                      P.op('pool', lambda e, e_=e_, vi=vi, XC5=XC5: e.tensor_add(
                          out=vnp[vi][:, :, :, e_, e_ * 64:(e_ + 1) * 64], in0=XC5[:, :, :, e_, :],
                          in1=LB4[:, :, e_, :].unsqueeze(1).to_broadcast([128, 2, 2, 64])),
                          reads=[rg, r_gp], writes=[r_vnp[vi]])
                  grp = p // 2
                  for jj in range(2):
                      j = (p % 2) * 2 + jj
                      for t in range(2):
                          sb_ = 4 + t
                          for e_ in range(2):
                              P.op('pe', lambda e, t=t, e_=e_, vi=vi, sb_=sb_, j=j, jj=jj: e.matmul(
                                  banks[sb_][:, j * 128:(j + 1) * 128], lhsT=vnp[vi][:, jj, t, e_, :], rhs=wsT[:, 2 * t + e_, :],
                                  start=(e_ == 0), stop=(e_ == 1)), reads=[r_vnp[vi], r_ws], writes=[rbank[sb_]])
                  if p % 2 == 1:
                      cols = slice(grp * 512, (grp + 1) * 512)
                      for t in range(2):
                          sb_ = 4 + t
                          P.op('dve', lambda e, t=t, sb_=sb_: e.tensor_add(
                              out=gtmp[t].rearrange("p (j c) -> p j c", j=4), in0=banks[sb_].rearrange("p (j c) -> p j c", j=4),
                              in1=bsb[:, t, :].unsqueeze(1).to_broadcast([128, 4, 128])), reads=[rbank[sb_], r_gp], writes=[r_gtmp[t]])
                          P.op('dve', lambda e, t=t, cols=cols: e.tensor_mul(out=catT[:, 4 + t, cols], in0=gtmp[t], in1=uT[:, t, cols]),
                               reads=[r_gtmp[t], r_uT[t][grp]], writes=[r_cat[4 + t][grp]])

              gm_stage_a(0)
              extra = {0: lambda: proj_ftile(2048, pz_eps[0]), 1: lambda: proj_ftile(2176, pz_eps[1]),
                       2: lambda: pool_part(0), 4: lambda: pool_part(1)}
              for p in range(8):
                  if p in extra:
                      extra[p]()
                  if p + 1 < 8:
                      gm_stage_a(p + 1)
                  gm_stage_b(p)

              if dbg == 'm4':
                  raise StopBuild()

              P.barrier()
              RX.reset(); RD.reset()
              qz = RX.bf(2 * S).rearrange("p (h t) -> p h t", h=2)
              kT_ = RX.bf(S)
              r_qk = [[Res() for _ in range(4)] for _ in range(2)]
              vpad = [RX.bf(16 * 2 * 128).rearrange("p (b h c) -> p b h c", b=16, h=2) for _ in range(2)]
              r_vpad = [Res(), Res()]
              acc = RX.f32(2 * S).rearrange("p (a t) -> p a t", a=2)
              r_acc = Res()
              PT = [RX.bf(512) for _ in range(4)]
              r_PT = [Res() for _ in range(4)]
              qraw = [RD.bf(512) for _ in range(2)]
              r_qraw = [Res(), Res()]
              t1 = [RD.f32(512) for _ in range(2)]
              t2 = [RD.f32(512) for _ in range(2)]
              r_t = [Res(), Res()]
              ropeC = RD.bf(S)
              ropeS = RD.bf(S)
              r_rope = Res()
              dma_in(ropeC, c_ropeC, 'd:gp', [r_rope])
              dma_in(ropeS, c_ropeS, 'd:gp', [r_rope])
              for vi in range(2):
                  P.op('pool', lambda e, vi=vi: e.memset(vpad[vi], 0.0), writes=[r_vpad[vi]])
              P.op('pool', lambda e: e.memset(qz, 0.0), writes=r_qk[0])
              vslot = [0]
              ui = [0]
              nli = [0]
              for hp in range(4):
                  for qi_ in range(2):
                      wtile, rw_ = load_ftile(l, qi_ * 512 + hp * 128)
                      for tt in range(4):
                          sl = (qi_ * 4 + tt) % 2
                          cols = slice(tt * 512, (tt + 1) * 512)
                          pq = (qi_ * 4 + tt) % 4
                          pp = 6 + sl
                          for k in range(8):
                              P.op('pe', lambda e, k=k, cols=cols, wtile=wtile, pq=pq: e.matmul(banks[pq], lhsT=wtile[:, k, :], rhs=xT[:, k, cols],
                                                                                               start=(k == 0), stop=(k == 7)),
                                   reads=[rw_] + r_xT[tt * 4:(tt + 1) * 4], writes=[rbank[pq]])
                          P.op('act', lambda e, sl=sl, pq=pq: e.copy(out=qraw[sl], in_=banks[pq]), reads=[rbank[pq]], writes=[r_qraw[sl]])
                          P.op('dve', lambda e, sl=sl, cols=cols: e.tensor_mul(out=t1[sl], in0=qraw[sl], in1=ropeC[:, cols]),
                               reads=[r_qraw[sl], r_rope], writes=[r_t[sl]])
                          P.op('pe', lambda e, sl=sl, pp=pp: e.matmul(banks[pp], lhsT=perm, rhs=qraw[sl], start=True, stop=True),
                               reads=[r_qraw[sl], r_const], writes=[rbank[pp]])
                          P.op('dve', lambda e, sl=sl, cols=cols, pp=pp: e.tensor_mul(out=t2[sl], in0=banks[pp], in1=ropeS[:, cols]),
                               reads=[rbank[pp], r_rope], writes=[r_t[sl]])
                          if qi_ == 1:
                              P.op('dve', lambda e, sl=sl, cols=cols: e.tensor_add(out=kT_[:, cols], in0=t1[sl], in1=t2[sl]),
                                   reads=[r_t[sl]], writes=[r_qk[1][tt]])
                          else:
                              for hh in range(2):
                                  rows = slice(hh * 64, (hh + 1) * 64)
                                  P.op('dve', lambda e, sl=sl, cols=cols, hh=hh, rows=rows: e.tensor_add(
                                      out=qz[rows, hh, cols], in0=t1[sl][rows, :], in1=t2[sl][rows, :]),
                                       reads=[r_t[sl]], writes=[r_qk[0][tt]])
                  if dbg == 'm3a':
                      raise StopBuild()
                  wv, r_wv = load_ftile(l, 1024 + hp * 128)
                  P.op('pool', lambda e: e.memset(acc, 0.0), writes=[r_acc])
                  def do_cfg_v(dil, L):
                      vs = vslot[0] % 2
                      vslot[0] += 1
                      VP = vpad[vs]
                      nbr = L // 128
                      for g4 in range(4):
                          for j in range(4):
                              blk = g4 * 4 + j
                              r_, jb = blk // nbr, blk % nbr
                              t0 = r_ + dil * jb * 128
                              tsl = slice(t0, t0 + dil * 127 + 1, dil)
                              rd = r_xT if dil > 1 else [r_xT[blk]]
                              for k in range(8):
                                  P.op('pe', lambda e, k=k, tsl=tsl, j=j, wv=wv, vb=6 + g4 % 2: e.matmul(banks[vb][:, j * 128:(j + 1) * 128], lhsT=xT[:, k, tsl],
                                                                                 rhs=wv[:, k, :], start=(k == 0), stop=(k == 7)),
                                       reads=[r_wv] + rd, writes=[rbank[6 + g4 % 2]])
                          b6 = banks[6 + g4 % 2].rearrange("p (j h c) -> p j h c", j=4, h=2)
                          for h in range(2):
                              P.op('act', lambda e, g4=g4, h=h, VP=VP, b6=b6: e.copy(
                                  out=VP[:, g4 * 4:(g4 + 1) * 4, h, h * 64:(h + 1) * 64], in_=b6[:, :, h, :]),
                                  reads=[rbank[6 + g4 % 2]], writes=[r_vpad[vs]])
                      return VP, vs, nbr

                  def do_cfg_units(dil, L, VP, vs, nbr):
                      allunits = []
                      for r_ in range(dil):
                          if L == 128:
                              units = [(0, 128, [(0, 768)])]
                          else:
                              units = [(0, 64, [(0, 512)])]
                              for i in range(L // 128 - 1):
                                  units.append((128 * i + 64, 128, [(i, 0), (i + 1, 128)]))
                              units.append((L - 64, 64, [(L // 128 - 1, 640)]))
                          for (qa, nq, kbs) in units:
                              allunits.append((r_, qa, nq, kbs))

                      def rcols(r_, a, n, dil=dil):
                          t0 = r_ + dil * a
                          return slice(t0, t0 + dil * (n - 1) + 1, dil)

                      def stage_s(un):
                          r_, qa, nq, kbs = un
                          u_ = ui[0]
                          ui[0] += 1
                          sbk = u_ % 4
                          nk = len(kbs)
                          tot = 2 * nk * nq
                          m0 = kbs[0][1]
                          mbase = 0 if nk == 2 else m0
                          qc_ = rcols(r_, qa, nq)
                          for hh in range(2):
                              for ki_, (kb, _) in enumerate(kbs):
                                  off = (hh * nk + ki_) * nq
                                  kc_ = rcols(r_, kb * 128, 128)
                                  P.op('pe', lambda e, hh=hh, kc_=kc_, qc_=qc_, off=off, nq=nq, sbk=sbk: e.matmul(
                                      banks[sbk][:, off:off + nq], lhsT=kT_[:, kc_], rhs=qz[:, hh, qc_],
                                      start=True, stop=True), reads=r_qk[0] + r_qk[1], writes=[rbank[sbk]])
                          P.op('act', lambda e, sbk=sbk, tot=tot: e.activation(out=PT[sbk][:, 0:tot], in_=banks[sbk][:, 0:tot],
                                                                              func=AF.Exp, scale=0.125),
                               reads=[rbank[sbk]], writes=[r_PT[sbk]])
                          P.op('pool' if u_ % 2 == 0 else 'dve', lambda e, sbk=sbk, tot=tot, mbase=mbase: e.tensor_mul(
                              out=PT[sbk][:, 0:tot], in0=PT[sbk][:, 0:tot], in1=masks[:, mbase:mbase + tot]),
                               reads=[r_PT[sbk], r_const], writes=[r_PT[sbk]])
                          return sbk

                      pvst = {'col': 0, 'first': 0, 'nbk': 4}

                      def stage_pv(un, sbk, VP=VP, vs=vs, nbr=nbr, L=L):
                          r_, qa, nq, kbs = un
                          nk = len(kbs)
                          if pvst['col'] == 0:
                              nli[0] += 1
                              pvst['nbk'] = 4 + nli[0] % 2
                              pvst['first'] = qa
                          nbk = pvst['nbk']
                          col = pvst['col']
                          n_mm = 2 * nk
                          i_mm = 0
                          for hh in range(2):
                              for ki_, (kb, _) in enumerate(kbs):
                                  off = (hh * nk + ki_) * nq
                                  blk = r_ * nbr + kb
                                  P.op('pe', lambda e, hh=hh, blk=blk, off=off, nq=nq, col=col, i_mm=i_mm, n_mm=n_mm, nbk=nbk, sbk=sbk: e.matmul(
                                      banks[nbk][:, col:col + nq], lhsT=VP[:, blk, hh, :], rhs=PT[sbk][:, off:off + nq],
                                      start=(i_mm == 0), stop=(i_mm == n_mm - 1)), reads=[r_PT[sbk], r_vpad[vs]], writes=[rbank[nbk]])
                                  i_mm += 1
                          i_mm = 0
                          for hh in range(2):
                              for ki_, (kb, _) in enumerate(kbs):
                                  off = (hh * nk + ki_) * nq
                                  P.op('pe', lambda e, hh=hh, off=off, nq=nq, col=col, i_mm=i_mm, n_mm=n_mm, nbk=nbk, sbk=sbk: e.matmul(
                                      banks[nbk][:, 256 + col:256 + col + nq], lhsT=onesh[:, hh, :], rhs=PT[sbk][:, off:off + nq],
                                      start=(i_mm == 0), stop=(i_mm == n_mm - 1)), reads=[r_PT[sbk], r_const], writes=[rbank[nbk]])
                                  i_mm += 1
                          col += nq
                          is_last = (qa + nq == L)
                          if col + 128 > 256 or is_last:
                              acols = rcols(r_, pvst['first'], col)
                              P.op('dve', lambda e, acols=acols, col=col, nbk=nbk: e.tensor_add(
                                  out=acc[:, :, acols], in0=acc[:, :, acols],
                                  in1=banks[nbk].rearrange("p (a c) -> p a c", a=2)[:, :, 0:col]),
                                   reads=[rbank[nbk], r_acc], writes=[r_acc])
                              col = 0
                          pvst['col'] = col

                      LA = 3
                      sb_of = {}
                      for i in range(len(allunits) + LA):
                          if i < len(allunits):
                              sb_of[i] = stage_s(allunits[i])
                          if i - LA >= 0:
                              stage_pv(allunits[i - LA], sb_of[i - LA])
                      if dbg == 'm3b':
                          raise StopBuild()

                  cfgs = ((1, 2048), (4, 512), (16, 128))
                  vinfo = do_cfg_v(*cfgs[0])
                  for ci in range(3):
                      nxt = do_cfg_v(*cfgs[ci + 1]) if ci + 1 < 3 else None
                      do_cfg_units(cfgs[ci][0], cfgs[ci][1], *vinfo)
                      vinfo = nxt
                  P.op('act', lambda e: e.activation(out=acc[:, 1, :], in_=acc[:, 1, :], func=AF.Ln), reads=[r_acc], writes=[r_acc])
                  P.op('act', lambda e: e.activation(out=acc[:, 1, :], in_=acc[:, 1, :], func=AF.Exp, scale=-1.0), reads=[r_acc], writes=[r_acc])
                  P.op('dve', lambda e, hp=hp: e.tensor_mul(out=catT[:, hp, :], in0=acc[:, 0, :], in1=acc[:, 1, :]), reads=[r_acc], writes=r_cat[hp])

              if dbg == 'm3':
                  raise StopBuild()

              P.barrier()
              RX.reset(); RD.reset()
              x1 = RX.f32(NTB * D).rearrange("p (t c) -> p t c", t=NTB)
              r_x1 = [Res() for _ in range(NTB)]
              wout = RD.bf(8 * 1024).rearrange("p (k c) -> p k c", k=8)
              r_wout = Res()
              xin = [RD.f32(1024) for _ in range(2)]
              r_xin = [Res(), Res()]
              xr32 = [RD.f32(1024).rearrange("p (k c) -> p k c", k=8) for _ in range(2)]
              r_xr = [Res(), Res()]
              g1b = RD.f32(1024)
              b1b = RD.f32(1024)
              r_g1 = Res()
              dma_in(g1b, ln1_g[l].partition_broadcast(128), 'd:gp', [r_g1])
              dma_in(b1b, ln1_b[l].partition_broadcast(128), 'd:gp', [r_g1])
              wo_ = w_out[l].rearrange("(k p) c -> p k c", p=128)
              for k in range(8):
                  for h in range(2):
                      stage_cast(wo_[:, k, h * 512:(h + 1) * 512], wout[:, k, h * 512:(h + 1) * 512], [r_wout])
              def m5_a(tb):
                  sl = tb % 2
                  dma_in(xin[sl], x_src[tb * 128:(tb + 1) * 128, :], 'd:xin%d' % sl, [r_xin[sl]],
                         reads=[r_scr] if l > 0 else ())
                  for h in range(2):
                      ob = 2 * sl + h
                      for k in range(8):
                          P.op('pe', lambda e, k=k, h=h, ob=ob, tb=tb: e.matmul(banks[ob], lhsT=catT[:, k, tok_cols(tb)],
                                                                                rhs=wout[:, k, h * 512:(h + 1) * 512],
                                                                                start=(k == 0), stop=(k == 7)),
                               reads=[r_cat[k][tb // 4], r_wout], writes=[rbank[ob]])
                      P.op('dve', lambda e, h=h, ob=ob, tb=tb, sl=sl: e.scalar_tensor_tensor(
                          out=x1[:, tb, h * 512:(h + 1) * 512], in0=xin[sl][:, h * 512:(h + 1) * 512], scalar=ALPHA,
                          in1=banks[ob], op0=ALU.mult, op1=ALU.add), reads=[r_xin[sl], rbank[ob]], writes=[r_x1[tb]])

              def m5_a2(tb):
                  ln_stats(x1[:, tb, :], r_x1[tb], tb % 2)

              def m5_b1(tb):
                  sl = tb % 2
                  ln_apply(x1[:, tb, :], r_x1[tb], g1b, b1b, r_g1, sl)
                  for h in range(2):
                      tbk = 4 + h
                      for k4 in range(4):
                          k = h * 4 + k4
                          P.op('pe', lambda e, k=k, k4=k4, tbk=tbk, tb=tb: e.transpose(
                              out=banks[tbk][:, k4 * 128:(k4 + 1) * 128], in_=x1[:, tb, k * 128:(k + 1) * 128], identity=ident32),
                              reads=[r_x1[tb], r_const], writes=[rbank[tbk]])
                      b3 = banks[tbk].rearrange("p (k c) -> p k c", k=4)
                      P.op('dve', lambda e, h=h, sl=sl, b3=b3: e.tensor_copy(out=xr32[sl][:, h * 4:(h + 1) * 4, :], in_=b3),
                           reads=[rbank[tbk]], writes=[r_xr[sl]])
                      P.op('act', lambda e, h=h, tb=tb, sl=sl: e.copy(out=xT[:, h * 4:(h + 1) * 4, tok_cols(tb)], in_=xr32[sl][:, h * 4:(h + 1) * 4, :]),
                           reads=[r_xr[sl]], writes=[r_xT[tb]])

              def m5_b2(tb):
                  sl = tb % 2
                  for k in range(8):
                      P.op('pe', lambda e, k=k, sl=sl: e.matmul(banks[6][:, 0:16], lhsT=xr32[sl][:, k, :], rhs=rw32[:, k, :],
                                                                start=(k == 0), stop=(k == 7)),
                           reads=[r_xr[sl], r_const], writes=[rbank[6]])
                  P.op('act', lambda e, tb=tb: e.activation(out=scores[:, tb, :], in_=banks[6][:, 0:16], func=AF.Exp, scale=-1.0),
                       reads=[rbank[6]], writes=[r_scores])
                  P.op('act', lambda e, tb=tb: e.mul(out=x1[:, tb, :], in_=x1[:, tb, :], mul=ALPHA),
                       reads=[r_x1[tb]], writes=[r_x1[tb]])

              for i in range(-3, NTB):
                  if 0 <= i + 3 < NTB:
                      m5_a(i + 3)
                  if 0 <= i + 2 < NTB:
                      m5_a2(i + 2)
                  if 0 <= i + 1 < NTB:
                      m5_b1(i + 1)
                  if 0 <= i:
                      m5_b2(i)

              sc2 = scores.rearrange("p t e -> p (t e)")
              P.op('dve', lambda e: e.tensor_scalar_add(out=sc2, in0=sc2, scalar1=1.0), reads=[r_scores], writes=[r_scores])
              P.op('dve', lambda e: e.reciprocal(out=sc2, in_=sc2), reads=[r_scores], writes=[r_scores])
              B3 = rt_b.rearrange("p (a c) -> p a c", c=4)
              E3 = rt_e.rearrange("p (a c) -> p a c", c=4)
              B23 = rt_b2.rearrange("p (a c) -> p a c", c=4)
              SL3 = rt_sel.rearrange("p (a c) -> p a c", c=4)
              rr = [r_rt]
              P.op('dve', lambda e: e.tensor_add(out=rt_b.rearrange("p (t e) -> p t e", t=16), in0=scores,
                                                 in1=rbt.unsqueeze(1).to_broadcast([128, 16, 16])), reads=[r_scores, r_const], writes=rr)
              P.op('dve', lambda e: e.reduce_max(out=rt_m1, in_=B3, axis=AX.X), reads=rr, writes=rr)
              P.op('dve', lambda e: e.tensor_tensor(out=E3, in0=B3, in1=rt_m1.unsqueeze(2).to_broadcast([128, 64, 4]), op=ALU.is_equal),
                   reads=rr, writes=rr)
              P.op('dve', lambda e: e.scalar_tensor_tensor(out=rt_b2, in0=rt_e, scalar=-1e9, in1=rt_b, op0=ALU.mult, op1=ALU.add),
                   reads=rr, writes=rr)
              P.op('dve', lambda e: e.reduce_max(out=rt_m2, in_=B23, axis=AX.X), reads=rr, writes=rr)
              P.op('dve', lambda e: e.tensor_add(out=rt_gs, in0=rt_m1, in1=rt_m2), reads=rr, writes=rr)
              GS3 = rt_gs.rearrange("p (t g) -> p t g", g=4)
              P.op('dve', lambda e: e.reduce_max(out=rt_gm, in_=GS3, axis=AX.X), reads=rr, writes=rr)
              P.op('dve', lambda e: e.tensor_tensor(out=rt_goh.rearrange("p (t g) -> p t g", g=4), in0=GS3,
                                                    in1=rt_gm.unsqueeze(2).to_broadcast([128, 16, 4]), op=ALU.is_equal), reads=rr, writes=rr)
              P.op('dve', lambda e: e.tensor_tensor(out=SL3, in0=B3, in1=rt_m2.unsqueeze(2).to_broadcast([128, 64, 4]), op=ALU.is_ge),
                   reads=rr, writes=rr)
              P.op('dve', lambda e: e.tensor_mul(out=SL3, in0=SL3, in1=rt_goh.unsqueeze(2).to_broadcast([128, 64, 4])), reads=rr, writes=rr)
              P.op('dve', lambda e: e.tensor_mul(out=rt_sel, in0=rt_sel, in1=sc2), reads=rr + [r_scores], writes=rr)
              P.op('dve', lambda e: e.reduce_sum(out=rt_den, in_=rt_sel.rearrange("p (t e) -> p t e", t=16), axis=AX.X), reads=rr, writes=rr)
              P.op('dve', lambda e: e.reciprocal(out=rt_den, in_=rt_den), reads=rr, writes=rr)
              P.op('dve', lambda e: e.tensor_mul(out=gates, in0=rt_sel.rearrange("p (t e) -> p t e", t=16),
                                                 in1=rt_den.unsqueeze(2).to_broadcast([128, 16, 16])), reads=rr, writes=[r_gates])

              if dbg == 'm5':
                  raise StopBuild()

              P.barrier()
              RC.reset(); RD.reset()
              wgu = [RD.bf(8 * 1024).rearrange("p (k c) -> p k c", k=8) for _ in range(2)]
              wd = [RD.bf(4 * 1024).rearrange("p (k c) -> p k c", k=4) for _ in range(2)]
              r_wgu = [Res(), Res()]
              r_wd = [Res(), Res()]
              hbuf = [RC.bf(4 * 512).rearrange("p (k c) -> p k c", k=4) for _ in range(2)]
              r_h = [Res(), Res()]
              sg = [RC.f32(512) for _ in range(2)]
              r_sg = [Res(), Res()]
              fi = [0]
              yi = [0]
              NEXP = 16 if dbg != 'e1' else 1
              def e_load(ex):
                  ws_ = ex % 2
                  ceng = 'act' if ex == 0 else 'pool'
                  wg_v = w_gate[l, ex].rearrange("(k p) f -> p k f", p=128)
                  wu_v = w_up[l, ex].rearrange("(k p) f -> p k f", p=128)
                  wd_v = w_down[l, ex].rearrange("(k p) f -> p k f", p=128)
                  for k in range(8):
                      stage_cast(wg_v[:, k, :], wgu[ws_][:, k, 0:512], [r_wgu[ws_]], eng=ceng)
                      stage_cast(wu_v[:, k, :], wgu[ws_][:, k, 512:1024], [r_wgu[ws_]], eng=ceng)
                  for k in range(4):
                      for h in range(2):
                          stage_cast(wd_v[:, k, h * 512:(h + 1) * 512], wd[ws_][:, k, h * 512:(h + 1) * 512], [r_wd[ws_]], eng=ceng)

              def e_gu(j, fcs):
                  ex, tt = j // 4, j % 4
                  ws_ = ex % 2
                  cols = slice(tt * 512, (tt + 1) * 512)
                  hs = j % 2
                  H = hbuf[hs]
                  for fc in fcs:
                      f_ = fi[0] % 2
                      fi[0] += 1
                      gb, ub = 2 * f_, 2 * f_ + 1
                      for k in range(8):
                          P.op('pe', lambda e, k=k, fc=fc, gb=gb, cols=cols, ws_=ws_: e.matmul(
                              banks[gb], lhsT=wgu[ws_][:, k, fc * 128:(fc + 1) * 128], rhs=xT[:, k, cols],
                              start=(k == 0), stop=(k == 7)), reads=[r_wgu[ws_]] + r_xT[tt * 4:(tt + 1) * 4], writes=[rbank[gb]])
                      for k in range(8):
                          P.op('pe', lambda e, k=k, fc=fc, ub=ub, cols=cols, ws_=ws_: e.matmul(
                              banks[ub], lhsT=wgu[ws_][:, k, 512 + fc * 128:512 + (fc + 1) * 128], rhs=xT[:, k, cols],
                              start=(k == 0), stop=(k == 7)), reads=[r_wgu[ws_]] + r_xT[tt * 4:(tt + 1) * 4], writes=[rbank[ub]])
                      P.op('act', lambda e, f_=f_, gb=gb: e.activation(out=sg[f_], in_=banks[gb], func=AF.Silu),
                           reads=[rbank[gb]], writes=[r_sg[f_]])
                      P.op('dve', lambda e, f_=f_, ub=ub, fc=fc, H=H: e.tensor_mul(out=H[:, fc, :], in0=sg[f_], in1=banks[ub]),
                           reads=[r_sg[f_], rbank[ub]], writes=[r_hf[hs][fc]])

              def e_down(j):
                  ex, tt = j // 4, j % 4
                  ws_ = ex % 2
                  hs = j % 2
                  H = hbuf[hs]
                  for ts in range(4):
                      tb = tt * 4 + ts
                      for h in range(2):
                          yb = 4 + yi[0] % 2
                          yi[0] += 1
                          for fc in range(4):
                              P.op('pe', lambda e, fc=fc, ts=ts, h=h, yb=yb, H=H, ws_=ws_: e.matmul(
                                  banks[yb], lhsT=H[:, fc, ts * 128:(ts + 1) * 128], rhs=wd[ws_][:, fc, h * 512:(h + 1) * 512],
                                  start=(fc == 0), stop=(fc == 3)), reads=[r_hf[hs][fc], r_wd[ws_]], writes=[rbank[yb]])
                          P.op('dve', lambda e, tb=tb, h=h, yb=yb, ex=ex: e.scalar_tensor_tensor(
                              out=x1[:, tb, h * 512:(h + 1) * 512], in0=banks[yb], scalar=gates[:, tb, ex:ex + 1],
                              in1=x1[:, tb, h * 512:(h + 1) * 512], op0=ALU.mult, op1=ALU.add),
                              reads=[rbank[yb], r_gates, r_x1[tb]], writes=[r_x1[tb]])

              r_hf = [[Res() for _ in range(4)] for _ in range(2)]
              NJ = NEXP * 4
              e_load(0)
              e_gu(0, range(4))
              for j in range(NJ):
                  if j % 4 == 0 and j // 4 + 1 < NEXP:
                      e_load(j // 4 + 1)
                  if j + 1 < NJ:
                      e_gu(j + 1, [0, 1])
                  e_down(j)
                  if j + 1 < NJ:
                      e_gu(j + 1, [2, 3])

              P.barrier()
              RD.reset()
              g2b = RD.f32(1024)
              b2b = RD.f32(1024)
              r_g2 = Res()
              ybf = [RD.bf(1024) for _ in range(2)]
              r_ybf = [Res(), Res()]
              dma_in(g2b, ln2_g[l].partition_broadcast(128), 'd:gp', [r_g2])
              dma_in(b2b, ln2_b[l].partition_broadcast(128), 'd:gp', [r_g2])
              r_scr = Res()
              def ln2_fin(tb):
                  sl = tb % 2
                  ln_apply(x1[:, tb, :], r_x1[tb], g2b, b2b, r_g2, sl)
                  if last:
                      ro = Res()
                      r_outs.append(ro)
                      P.dma('sp', lambda e, tb=tb, s=s: e.dma_start(out=out[s, tb * 128:(tb + 1) * 128, :], in_=x1[:, tb, :]),
                            'd:out%d' % sl, reads=[r_x1[tb]], writes=[ro])
                  else:
                      P.dma('sp', lambda e, tb=tb: e.dma_start(out=scr[tb * 128:(tb + 1) * 128, :], in_=x1[:, tb, :]),
                            'd:out%d' % sl, reads=[r_x1[tb]], writes=[r_scr])
                      to_xT_bf16(x1[:, tb, :], r_x1[tb], ybf[sl], r_ybf[sl], tb, 6 + sl)

              ln_stats(x1[:, 0, :], r_x1[0], 0)
              for tb in range(NTB):
                  if tb + 1 < NTB:
                      ln_stats(x1[:, tb + 1, :], r_x1[tb + 1], (tb + 1) % 2)
                  ln2_fin(tb)

    except StopBuild:
        pass

    if dbg is not None:
        P.barrier()
        if dbg == 'm4':
            for k in range(4, 8):
                tmpf = f32v(X_o, 2048)
                rt_ = Res()
                P.op('pool', lambda e, k=k: e.tensor_copy(out=tmpf, in_=catT[:, k, :]), writes=[rt_])
                rd_ = dump(tmpf, [rt_])
                P.barrier()
        elif dbg == 'm3':
            for k in range(0, 8):
                tmpf = f32v(D_o, 2048)
                rt_ = Res()
                P.op('pool', lambda e, k=k: e.tensor_copy(out=tmpf, in_=catT[:, k, :]), writes=[rt_])
                dump(tmpf, [rt_])
                P.barrier()
        elif dbg == 'm3a':
            for qi_ in range(2):
                tmpf = f32v(D_o, 2048)
                rt_ = Res()
                P.op('pool', lambda e, qi_=qi_: e.tensor_copy(out=tmpf, in_=(kT_ if qi_ == 1 else qz[:, 0, :])), writes=[rt_])
                dump(tmpf, [rt_])
                P.barrier()
        elif dbg == 'm3b':
            dump(acc[:, 0, :], [])
            dump(acc[:, 1, :], [])
            P.barrier()
        elif dbg in ('m5', 'e1'):
            dump(x1[:, 0, :], [])
            dump(x1[:, 15, :], [])
            dump(gates.rearrange("p t e -> p (t e)"), [])
            dump(scores.rearrange("p t e -> p (t e)"), [])
            P.barrier()
        P.barrier()
    P.barrier()
    P.emit(nc, st)
    st.close()
    return nc


_CONSTS = None


def kernel(**inputs):
    global _CONSTS
    if _CONSTS is None:
        _CONSTS = _consts()
    n = 8
    nc = build(2, 2)
    x = np.ascontiguousarray(inputs["x"], dtype=np.float32)
    shared = {}
    for k, v in inputs.items():
        if k == "x":
            continue
        a = np.ascontiguousarray(np.asarray(v, dtype=np.float32))
        if k in ("gmlp_ln_g", "gmlp_ln_b"):
            a = a.reshape(2, 256)
        shared[k] = a
    shared.update(_CONSTS)
    in_maps = []
    for c in range(n):
        m = dict(shared)
        m["x"] = x[2 * c:2 * c + 2]
        in_maps.append(m)
    res = run_bass_kernel_spmd(nc, in_maps, core_ids=list(range(n)))
    return np.concatenate([r["out"] for r in res.results], axis=0).astype(np.float32)
```
